# Optimizing a Trainium2 kernel written in Bass

```python
import math
import jax, jax.numpy as jnp
from jax import lax
import numpy as np

D_MODEL = 1024
BATCH = 1
SEQ = 16384
DEPTH = 1

HEAD_DIM = 64
SWA_HEADS = 8
SWA_KV_HEADS = 2
SWA_GROUP = SWA_HEADS // SWA_KV_HEADS
WINDOW = 128
FOX_HEADS = 8
BLOCK = 128
REL_BUCKETS = 32
REL_MAX_DIST = WINDOW
N_EXPERTS = 32
TOP_K = 4
D_FF = D_MODEL
SWIGLU_LIMIT = 7.0
SWIGLU_ALPHA = 1.702
RMS_EPS = 1e-5
N_MOD = 6

SWA_Q = SWA_HEADS * HEAD_DIM
SWA_KV = SWA_KV_HEADS * HEAD_DIM
FOX_W = FOX_HEADS * HEAD_DIM
IN_SPLITS = (SWA_Q, SWA_KV, SWA_KV, FOX_W, FOX_W, FOX_W, FOX_HEADS, D_MODEL, D_MODEL)
D_IN = sum(IN_SPLITS)

kernel_name = "gated_hybrid_swa_fox_moe_adaln"


def rms_norm(x, g):
    xf = x.astype(jnp.float32)
    y = xf * lax.rsqrt(jnp.mean(xf * xf, axis=-1, keepdims=True) + RMS_EPS)
    return (y * g.astype(jnp.float32)).astype(x.dtype)


def modulate(h, shift, scale):
    return h * (1.0 + scale[:, None, :]) + shift[:, None, :]


def t5_causal_buckets(dist):
    n = jnp.maximum(dist, 0)
    max_exact = REL_BUCKETS // 2
    nf = jnp.maximum(n, 1).astype(jnp.float32)
    large = max_exact + (jnp.log(nf / max_exact) / math.log(REL_MAX_DIST / max_exact)
                         * (REL_BUCKETS - max_exact)).astype(jnp.int32)
    large = jnp.minimum(large, REL_BUCKETS - 1)
    return jnp.where(n < max_exact, n, large)


def sliding_window_gqa(q, k, v, sinks, rel_table):
    B, S = q.shape[0], q.shape[1]
    nb = S // BLOCK
    qb = q.reshape(B, nb, BLOCK, SWA_KV_HEADS, SWA_GROUP, HEAD_DIM)
    kb = k.reshape(B, nb, BLOCK, SWA_KV_HEADS, HEAD_DIM)
    vb = v.reshape(B, nb, BLOCK, SWA_KV_HEADS, HEAD_DIM)
    shift = lambda t: jnp.concatenate([jnp.zeros_like(t[:, :1]), t[:, :-1]], axis=1)
    kk = jnp.concatenate([shift(kb), kb], axis=2)
    vv = jnp.concatenate([shift(vb), vb], axis=2)
    scores = jnp.einsum('bnqhgd,bnkhd->bnhgqk', qb, kk).astype(jnp.float32) * (HEAD_DIM ** -0.5)

    qi = jnp.arange(BLOCK)[:, None]
    kj = jnp.arange(2 * BLOCK)[None, :]
    dist = BLOCK + qi - kj
    bias = rel_table.astype(jnp.float32)[t5_causal_buckets(dist)]
    bias = bias.transpose(2, 0, 1).reshape(SWA_KV_HEADS, SWA_GROUP, BLOCK, 2 * BLOCK)
    band = (dist >= 0) & (dist < WINDOW)
    blk = jnp.arange(nb)[:, None, None]
    key_ok = (blk * BLOCK - BLOCK + kj[None]) >= 0
    mask = band[None] & key_ok
    scores = jnp.where(mask[None, :, None, None], scores + bias, -jnp.inf)

    sink = jnp.broadcast_to(sinks.astype(jnp.float32).reshape(SWA_KV_HEADS, SWA_GROUP)[None, None, :, :, None, None],
                            scores.shape[:-1] + (1,))
    p = jax.nn.softmax(jnp.concatenate([scores, sink], axis=-1), axis=-1)[..., :-1]
    out = jnp.einsum('bnhgqk,bnkhd->bnqhgd', p.astype(v.dtype), vv)
    return out.reshape(B, S, SWA_Q)


def forgetting_attention(q, k, v, logf):
    B, S = q.shape[0], q.shape[1]
    nb = S // BLOCK
    cum_t = lax.cumsum(logf.astype(jnp.float32), axis=1).transpose(0, 2, 1)
    kpos = jnp.arange(S)

    def one_block(i):
        start = i * BLOCK
        qi = lax.dynamic_slice_in_dim(q, start, BLOCK, axis=1)
        ci = lax.dynamic_slice_in_dim(cum_t, start, BLOCK, axis=2)
        s = (jnp.einsum('bqhd,bkhd->bhqk', qi, k).astype(jnp.float32) * (HEAD_DIM ** -0.5)
             + ci[..., None] - cum_t[:, :, None, :])
        qpos = start + jnp.arange(BLOCK)
        s = jnp.where(qpos[:, None] >= kpos[None, :], s, -jnp.inf)
        p = jax.nn.softmax(s, axis=-1).astype(v.dtype)
        return jnp.einsum('bhqk,bkhd->bqhd', p, v)

    out = lax.map(one_block, jnp.arange(nb))
    return out.transpose(1, 0, 2, 3, 4).reshape(B, S, FOX_W)


def moe_ffn(u, w_router, b_router, w_e1, b_e1, w_e2, b_e2):
    logits = (u @ w_router).astype(jnp.float32) + b_router.astype(jnp.float32)
    top_v, top_i = lax.top_k(logits, TOP_K)
    top_w = jax.nn.softmax(top_v, axis=-1)
    combine = jnp.sum(jax.nn.one_hot(top_i, N_EXPERTS, dtype=jnp.float32) * top_w[..., None], axis=-2)
    combine = combine.astype(u.dtype)
    out = jnp.zeros_like(u)
    for e in range(N_EXPERTS):
        h = u @ w_e1[e] + b_e1[e]
        glu = jnp.minimum(h[..., ::2], SWIGLU_LIMIT)
        lin = jnp.clip(h[..., 1::2], -SWIGLU_LIMIT, SWIGLU_LIMIT)
        a = glu * jax.nn.sigmoid(SWIGLU_ALPHA * glu) * (lin + 1.0)
        out = out + combine[..., e:e + 1] * (a @ w_e2[e] + b_e2[e])
    return out


def setup_inputs(seed: int = 0) -> dict:
    key = jax.random.key(seed)
    ks = jax.random.split(key, 20)
    f32 = jnp.float32
    nrm = lambda k, shape, s: jax.random.normal(k, shape, f32) * s
    return {
        "x": nrm(ks[0], (BATCH, SEQ, D_MODEL), 1.0),
        "c": nrm(ks[1], (BATCH, D_MODEL), 1.0),
        "w_ada": nrm(ks[2], (DEPTH, D_MODEL, N_MOD * D_MODEL), D_MODEL ** -0.5),
        "b_ada": nrm(ks[3], (DEPTH, N_MOD * D_MODEL), 0.02),
        "g_mix": 1.0 + nrm(ks[4], (DEPTH, D_MODEL), 0.02),
        "w_in": nrm(ks[5], (DEPTH, D_MODEL, D_IN), D_MODEL ** -0.5),
        "b_forget": 2.0 + nrm(ks[6], (DEPTH, FOX_HEADS), 0.5),
        "sinks": nrm(ks[7], (DEPTH, SWA_HEADS), 0.5),
        "rel_bias": nrm(ks[8], (REL_BUCKETS, SWA_HEADS), 0.1),
        "w_proj_a": nrm(ks[9], (DEPTH, SWA_Q, D_MODEL), SWA_Q ** -0.5),
        "w_proj_b": nrm(ks[10], (DEPTH, FOX_W, D_MODEL), FOX_W ** -0.5),
        "w_out": nrm(ks[11], (DEPTH, D_MODEL, D_MODEL), D_MODEL ** -0.5),
        "g_ffn": 1.0 + nrm(ks[12], (DEPTH, D_MODEL), 0.02),
        "w_router": nrm(ks[13], (DEPTH, D_MODEL, N_EXPERTS), D_MODEL ** -0.5),
        "b_router": nrm(ks[14], (DEPTH, N_EXPERTS), 0.01),
        "w_e1": nrm(ks[15], (DEPTH, N_EXPERTS, D_MODEL, 2 * D_FF), D_MODEL ** -0.5),
        "b_e1": nrm(ks[16], (DEPTH, N_EXPERTS, 2 * D_FF), 0.02),
        "w_e2": nrm(ks[17], (DEPTH, N_EXPERTS, D_FF, D_MODEL), D_FF ** -0.5),
        "b_e2": nrm(ks[18], (DEPTH, N_EXPERTS, D_MODEL), 0.02),
        "g_final": 1.0 + nrm(ks[19], (D_MODEL,), 0.02),
    }


def reference(x, c, w_ada, b_ada, g_mix, w_in, b_forget, sinks, rel_bias, w_proj_a, w_proj_b, w_out,
              g_ffn, w_router, b_router, w_e1, b_e1, w_e2, b_e2, g_final):
    B, S = x.shape[0], x.shape[1]
    split_idx = [int(v) for v in np.cumsum(IN_SPLITS)[:-1]]
    c_act = jax.nn.silu(c)
    for l in range(DEPTH):
        mod = c_act @ w_ada[l] + b_ada[l]
        sh_m, sc_m, gt_m, sh_f, sc_f, gt_f = jnp.split(mod, N_MOD, axis=-1)

        u = modulate(rms_norm(x, g_mix[l]), sh_m, sc_m)
        z = u @ w_in[l]
        qa, ka, va, qb, kb, vb, fb, ga, gb = jnp.split(z, split_idx, axis=-1)
        ya = sliding_window_gqa(qa.reshape(B, S, SWA_HEADS, HEAD_DIM),
                                ka.reshape(B, S, SWA_KV_HEADS, HEAD_DIM),
                                va.reshape(B, S, SWA_KV_HEADS, HEAD_DIM),
                                sinks[l], rel_bias)
        logf = jax.nn.log_sigmoid(fb.astype(jnp.float32) + b_forget[l].astype(jnp.float32))
        yb = forgetting_attention(qb.reshape(B, S, FOX_HEADS, HEAD_DIM),
                                  kb.reshape(B, S, FOX_HEADS, HEAD_DIM),
                                  vb.reshape(B, S, FOX_HEADS, HEAD_DIM), logf)
        merged = jax.nn.sigmoid(ga) * (ya @ w_proj_a[l]) + jax.nn.sigmoid(gb) * (yb @ w_proj_b[l])
        x = x + gt_m[:, None, :] * (merged @ w_out[l])

        u = modulate(rms_norm(x, g_ffn[l]), sh_f, sc_f)
        x = x + gt_f[:, None, :] * moe_ffn(u, w_router[l], b_router[l], w_e1[l], b_e1[l], w_e2[l], b_e2[l])
    return rms_norm(x, g_final)
```

```python
import numpy as np
from contextlib import ExitStack
import concourse.bass as bass
import concourse.mybir as mybir
from concourse.bass_utils import run_bass_kernel_spmd

F32 = mybir.dt.float32
BF16 = mybir.dt.bfloat16
I32 = mybir.dt.int32
AF = mybir.ActivationFunctionType
ALU = mybir.AluOpType
AX = mybir.AxisListType


class Buf:
    __slots__ = ("name", "w", "r")

    def __init__(self, name=""):
        self.name = name
        self.w = None
        self.r = []


class Tn:
    def __init__(self, t, name=""):
        self.t = t
        self.b = Buf(name)

    def __getitem__(self, k):
        return self.t[k]


class Prog:
    ENG = ("pe", "act", "dve", "pool", "sp")

    def __init__(self, nc, es):
        self.nc = nc
        self.es = es
        self.sems = {e: es.enter_context(nc.semaphore("s_" + e)) for e in self.ENG}
        self.cnt = {e: 0 for e in self.ENG}
        self.chans = {}
        self.touched = set()
        self.streams = {e: [] for e in self.ENG}
        self.nphase = 0

    def chan(self, name):
        if name not in self.chans:
            self.chans[name] = [self.es.enter_context(self.nc.semaphore("c_" + name)), 0]
        return self.chans[name]

    def op(self, eng, fn, R=(), W=(), dma=None):
        deps = set()
        for b in R:
            b = b.b if isinstance(b, Tn) else b
            if b.w is not None:
                deps.add(b.w)
        for b in W:
            b = b.b if isinstance(b, Tn) else b
            if b.w is not None:
                deps.add(b.w)
            for t in b.r:
                deps.add(t)
        st = self.streams[eng]
        if dma is None:
            tok = ("e", eng, len(st))
            if eng == "pe":
                deps = {d for d in deps if not (d[0] == "e" and d[1] == "pe")}
        else:
            if eng == "pool":
                self.pool_rr = getattr(self, "pool_rr", 0) + 1
                dma = "pq%d" % (self.pool_rr % 24)
                ch = self.chan(dma)
                if ch[1] > 0:
                    deps.add(("d", dma, ch[1] - 1))
            ch = self.chan(dma)
            tok = ("d", dma, ch[1])
            ch[1] += 1
        st.append({"fn": fn, "deps": deps, "sig": False, "dma": dma})
        for b in R:
            b = b.b if isinstance(b, Tn) else b
            b.r.append(tok)
            self.touched.add(b)
        for b in W:
            b = b.b if isinstance(b, Tn) else b
            b.w = tok
            b.r = []
            self.touched.add(b)
        return tok

    def flush(self):
        nc = self.nc
        streams = self.streams
        for e in self.ENG:
            for ins in streams[e]:
                for d in ins["deps"]:
                    if d[0] == "e":
                        streams[d[1]][d[2]]["sig"] = True
        for e in self.ENG:
            for ins in reversed(streams[e]):
                if ins["dma"] is None:
                    ins["sig"] = True
                    break
        val = {}
        for e in self.ENG:
            c = self.cnt[e]
            for i, ins in enumerate(streams[e]):
                if ins["dma"] is None and ins["sig"]:
                    c += 1
                val[(e, i)] = c
            self.cnt[e] = c
        final_cnt = dict(self.cnt)
        final_ch = {k: 16 * v[1] for k, v in self.chans.items()}
        sems = self.sems
        chans = self.chans

        def emit(e, eng):
            waited = {}
            for ins in streams[e]:
                for d in sorted(ins["deps"]):
                    if d[0] == "e":
                        key = ("e", d[1])
                        v = val[(d[1], d[2])]
                        sem = sems[d[1]]
                    else:
                        key = ("d", d[1])
                        v = 16 * (d[2] + 1)
                        sem = chans[d[1]][0]
                    if waited.get(key, -1) >= v:
                        continue
                    eng.wait_ge(sem, v)
                    waited[key] = v
                bi = ins["fn"](eng)
                if ins["dma"] is not None:
                    bi.then_inc(chans[ins["dma"]][0], 16)
                elif ins["sig"]:
                    bi.then_inc(sems[e], 1)
            for o in self.ENG:
                if o != e and final_cnt[o] > 0:
                    eng.wait_ge(sems[o], final_cnt[o])
            if final_cnt[e] > 0:
                eng.wait_ge(sems[e], final_cnt[e])
            for k, v in final_ch.items():
                if v > 0:
                    eng.wait_ge(chans[k][0], v)

        with nc.Block() as blk:
            @blk.tensor
            def _(eng):
                emit("pe", eng)

            @blk.scalar
            def _(eng):
                emit("act", eng)

            @blk.vector
            def _(eng):
                emit("dve", eng)

            @blk.gpsimd
            def _(eng):
                emit("pool", eng)

            @blk.sync
            def _(eng):
                emit("sp", eng)

        self.streams = {e: [] for e in self.ENG}
        for b in self.touched:
            b.w = None
            b.r = []
        self.touched = set()
        self.nphase += 1

    def mm(self, out, lhsT, rhs, start=True, stop=True, R=(), W=()):
        return self.op("pe", lambda e: e.matmul(out, lhsT, rhs, start=start, stop=stop), R, W)

    def tr(self, out, in_, ident, R=(), W=()):
        return self.op("pe", lambda e: e.transpose(out, in_, ident), R, W)

    def act(self, out, in_, func, R=(), W=(), **kw):
        return self.op("act", lambda e: e.activation(out, in_, func, **kw), R, W)

    def ts(self, eng, out, in0, s1, s2, op0, op1=None, R=(), W=(), **kw):
        if op1 is None:
            return self.op(eng, lambda e: e.tensor_single_scalar(out, in0, s1, op0), R, W)
        return self.op(eng, lambda e: e.tensor_scalar(out, in0, s1, s2, op0, op1, **kw), R, W)

    def tt(self, eng, out, in0, in1, op, R=(), W=()):
        return self.op(eng, lambda e: e.tensor_tensor(out, in0, in1, op), R, W)

    def stt(self, eng, out, in0, scalar, in1, op0, op1, R=(), W=()):
        return self.op(eng, lambda e: e.scalar_tensor_tensor(out, in0, scalar, in1, op0, op1), R, W)

    def cp(self, eng, out, in_, R=(), W=()):
        if eng == "act":
            return self.op("act", lambda e: e.activation(out, in_, AF.Copy), R, W)
        return self.op(eng, lambda e: e.tensor_copy(out, in_), R, W)

    def memset(self, eng, ap, v, W=()):
        return self.op(eng, lambda e: e.memset(ap, v), (), W)

    def dma(self, q, out, in_, chan, R=(), W=()):
        return self.op(q, lambda e: e.dma_start(out=out, in_=in_), R, W, dma=chan)


S = 16384
D = 1024
NBLK = 128
OWN = 16
NCORES = 8
EPS = 1e-5
NEG = -30000.0
BIG = 3.0e38
STAGE = 99
DEBUG = False


def build(stage=99, dense_experts=32):
    nc = bass.Bass("TRN2", target_bir_lowering=False)

    def din(name, shape, dt=F32):
        return nc.dram_tensor(name, list(shape), dt, kind="ExternalInput").ap()

    def dscr(name, shape, dt):
        kind = "ExternalOutput" if (DEBUG and name in ("X1",)) else "Internal"
        return nc.dram_tensor(name, list(shape), dt, kind=kind).ap()

    x_all = din("x_all", [S, D])
    x_own = din("x_own", [2048, D])
    x_halo = din("x_halo", [2048, D])
    c_t = din("c_t", [128, 8])
    w_ada = din("w_ada", [D, 6 * D])
    b_ada = din("b_ada", [1, 6 * D])
    g_mix = din("g_mix", [1, D])
    w_in = din("w_in", [D, 4360])
    b_forget = din("b_forget", [1, 8])
    sinks = din("sinks", [1, 8])
    swa_bias = din("swa_bias", [128, 2 * 8 * 128])
    swa_mask = din("swa_mask", [128, 2 * 128])
    w_pa = din("w_pa", [512, D])
    w_pb = din("w_pb", [512, D])
    w_out = din("w_out", [D, D])
    g_ffn = din("g_ffn", [1, D])
    w_router = din("w_router", [D, 32])
    b_router = din("b_router", [1, 32])
    w1g = din("w1g", [32, D, D])
    w1l = din("w1l", [32, D, D])
    w2 = din("w2", [32, D, D])
    b2 = din("b2", [32, D])
    g_final = din("g_final", [1, D])
    ident = din("ident", [128, 128])
    tri = din("tri", [128, 128])
    fmask = din("fmask", [128, 8 * 128])
    meta = din("meta", [128, 24])
    mconst = din("mconst", [128, 48 + 9])
    tris = din("tris", [128, 128])
    b1g_rows = din("b1g_rows", [4096, 8])
    b1l_rows = din("b1l_rows", [4096, 8])
    y = nc.dram_tensor("y", [2048, D], F32, kind="ExternalOutput").ap()

    Ks = dscr("Ks", [8, 64, S], BF16)
    Kc = dscr("Kc", [24, S], BF16)
    Vs = dscr("Vs", [8, 128, 128, 65], BF16)
    Qs = dscr("Qs", [8, 64, 2048], BF16)
    Qc = dscr("Qc", [24, 2048], BF16)
    YA = dscr("YA", [512, 2048], BF16)
    YB = dscr("YB", [8, 64, 2048], BF16)
    X1 = dscr("X1", [2048, D], F32)
    Xs = dscr("Xs", [48 * 512, D], BF16)
    Ys = dscr("Ys", [48 * 512, D], F32)
    dXs, dYs = Buf("Xs"), Buf("Ys")
    DBG = nc.dram_tensor("DBG", [128, 256], F32, kind="ExternalOutput").ap() if DEBUG else None
    dKs, dKc, dVs, dQs, dQc, dYA, dYB, dX1, dY = (Buf(n) for n in "Ks Kc Vs Qs Qc YA YB X1 Y".split())

    with ExitStack() as es:
        p = Prog(nc, es)

        def sb(name, shape, dt, st=es):
            return Tn(st.enter_context(nc.sbuf_tensor(name, shape, dt)), name)

        def ps(name, shape, dt, st=es):
            return Tn(st.enter_context(nc.psum_tensor(name, shape, dt)), name)

        PSA = [ps("psa%d" % i, [128, 512], F32) for i in range(6)]
        PST = [ps("pst%d" % i, [128, 1024], BF16) for i in range(2)]
        rot = {"psa": 0, "pst": 0, "n": 0}

        def psa():
            rot["psa"] += 1
            return PSA[rot["psa"] % 6]

        def pst():
            rot["pst"] += 1
            return PST[rot["pst"] % 2]

        idb = sb("idb", [128, 128], BF16)
        idf = sb("idf", [128, 128], F32)
        SHF = sb("SHF", [128, D], F32)
        GF = sb("GF", [128, D], F32)
        GTF = sb("GTF", [128, D], F32)
        GFIN = sb("GFIN", [128, D], F32)
        esink = sb("esink", [128, 8], F32)
        bfb = sb("bfb", [128, 4, 8], F32)
        brb = sb("brb", [128, 32], F32)
        epsc = sb("epsc", [128, 1], F32)
        onec = sb("onec", [128, 1], F32)
        metat = sb("metat", [128, 24], F32)
        onesf = sb("onesf", [128, 128], F32)
        esm = ExitStack()
        SHM = sb("SHM", [128, D], F32, esm)
        GM = sb("GM", [128, D], F32, esm)
        GTM = sb("GTM", [128, D], F32, esm)

        with ExitStack() as ph:
            cT = sb("cT", [128, 8], F32, ph)
            cact = sb("cact", [128, 8], F32, ph)
            onesb = sb("onesb", [128, 128], BF16, ph)
            cbs = sb("cbs", [128, 8, 128], BF16, ph)
            wada = [sb("wada%d" % i, [128, 8, 1024], BF16, ph) for i in range(2)]
            bada = sb("bada", [128, 6 * D], F32, ph)
            gmb = sb("gmb", [128, D], F32, ph)
            gfb = sb("gfb", [128, D], F32, ph)
            snk = sb("snk", [128, 8], F32, ph)
            p.dma("pool", idb[:], ident, "cast", W=[idb])
            p.dma("sp", idf[:], ident, "ld", W=[idf])
            p.dma("sp", cT[:], c_t, "ld", W=[cT])
            p.dma("sp", bada[:], b_ada.partition_broadcast(128), "ld", W=[bada])
            p.dma("sp", gmb[:], g_mix.partition_broadcast(128), "ld", W=[gmb])
            p.dma("sp", gfb[:], g_ffn.partition_broadcast(128), "ld", W=[gfb])
            p.dma("sp", GFIN[:], g_final.partition_broadcast(128), "ld", W=[GFIN])
            p.dma("sp", snk[:], sinks.partition_broadcast(128), "ld", W=[snk])
            for t4 in range(4):
                p.dma("sp", bfb[:, t4, :], b_forget.partition_broadcast(128), "ld", W=[bfb])
            p.dma("sp", brb[:], b_router.partition_broadcast(128), "ld", W=[brb])
            p.dma("sp", metat[:], meta, "ld", W=[metat])
            p.memset("dve", epsc[:], EPS, W=[epsc])
            p.memset("dve", onec[:], 1.0, W=[onec])
            p.memset("dve", onesf[:], 1.0, W=[onesf])
            p.memset("dve", onesb[:], 1.0, W=[onesb])
            p.act(esink[:], snk[:], AF.Exp, R=[snk], W=[esink])
            p.act(cact[:], cT[:], AF.Silu, R=[cT], W=[cact])
            for kc in range(8):
                p.ts("dve", cbs[:, kc, :], onesb[:], cact[:, kc:kc + 1], None, ALU.mult, R=[onesb, cact], W=[cbs])
            dst = [SHM, GM, GTM, SHF, GF, GTF]
            for n in range(6):
                wa = wada[n % 2]
                p.dma("pool", wa[:], w_ada[:, n * D:(n + 1) * D].rearrange("(k p) n -> p k n", p=128), "cast", W=[wa])
                for hf in range(2):
                    pa = psa()
                    for kc in range(8):
                        p.mm(pa[:], cbs[:, kc, :], wa[:, kc, hf * 512:(hf + 1) * 512], start=(kc == 0), stop=(kc == 7),
                             R=[cbs, wa], W=[pa])
                    p.tt("dve", dst[n][:, hf * 512:(hf + 1) * 512], pa[:], bada[:, n * D + hf * 512:n * D + (hf + 1) * 512],
                         ALU.add, R=[pa, bada], W=[dst[n]])
            p.stt("dve", GM[:], GM[:], 1.0, gmb[:], ALU.add, ALU.mult, R=[GM, gmb], W=[GM])
            p.stt("dve", GF[:], GF[:], 1.0, gfb[:], ALU.add, ALU.mult, R=[GF, gfb], W=[GF])
            p.flush()

        def mk_norm(ph, tag, nx=3, nt=2, nu=2):
            XT = [sb("xt%s%d" % (tag, i), [128, D], F32, ph) for i in range(nx)]
            TMP = [sb("tmp%s%d" % (tag, i), [128, D], F32, ph) for i in range(nt)]
            U = [sb("u%s%d" % (tag, i), [128, D], BF16, ph) for i in range(nu)]
            junk = sb("junk" + tag, [128, D], BF16, ph)
            SSq = [sb("ss%s%d" % (tag, i), [128, 1], F32, ph) for i in range(3)]
            RS = [sb("rs%s%d" % (tag, i), [128, 1], F32, ph) for i in range(3)]

            def norm_a1(src, srcbuf, G, SH):
                i = rot["n"]
                rot["n"] += 1
                xt, tmp, u, ss, rs = XT[i % nx], TMP[i % nt], U[i % nu], SSq[i % 3], RS[i % 3]
                p.dma("sp", xt[:], src, "ldx", R=srcbuf, W=[xt])
                p.memset("pool", ss[:], 0.0, W=[ss])
                p.act(junk[:], xt[:], AF.Square, R=[xt], W=[junk, ss], accum_out=ss[:])
                p.ts("dve", rs[:], ss[:], 1.0 / D, EPS, ALU.mult, ALU.add, R=[ss], W=[rs])
                p.act(rs[:], rs[:], AF.Sqrt, R=[rs], W=[rs])
                p.op("dve", lambda e: e.reciprocal(rs[:], rs[:]), R=[rs], W=[rs])
                p.stt("dve", tmp[:], xt[:], rs[:], G[:], ALU.mult, ALU.mult, R=[xt, rs, G], W=[tmp])
                p.tt("pool", u[:], tmp[:], SH[:], ALU.add, R=[tmp, SH], W=[u])
                return (xt, u)

            def norm_a2(hd, uT, col0, uTbuf=None):
                xt, u = hd
                pt = pst()
                for kc in range(8):
                    p.tr(pt[:, kc * 128:(kc + 1) * 128], u[:, kc * 128:(kc + 1) * 128], idb[:], R=[u, idb], W=[pt])
                p.cp("act", uT[:, :, col0:col0 + 128], pt[:].rearrange("p (k t) -> p k t", k=8), R=[pt],
                     W=[uT if uTbuf is None else uTbuf])
                return xt

            def norm_tile(src, srcbuf, G, SH, uT, col0, uTbuf=None):
                return norm_a2(norm_a1(src, srcbuf, G, SH), uT, col0, uTbuf)
            norm_tile.a1 = norm_a1
            norm_tile.a2 = norm_a2
            return norm_tile

        with ExitStack() as ph:
            norm_tile = mk_norm(ph, "a", 3, 2, 3)
            wkv = sb("wkv", [128, 8, 1032], BF16, ph)
            UT = [sb("uTa%d" % i, [128, 8, 512], BF16, ph) for i in range(2)]
            VSB = [sb("vsb%d" % i, [128, 8, 4, 65], BF16, ph) for i in range(2)]
            KSB = [sb("ksb%d" % i, [128, 512], BF16, ph) for i in range(2)]
            LF = sb("LF", [128, 128, 8], F32, ph)
            fa = sb("fa", [128, 32], F32, ph)
            fb_ = sb("fb_", [128, 32], F32, ph)
            fc = sb("fc", [128, 32], F32, ph)
            fd = sb("fd", [128, 32], F32, ph)
            psF = PSA[5]
            p.dma("pool", wkv[:], w_in[:, 1280:2312].rearrange("(k p) n -> p k n", p=128), "cast", W=[wkv])
            for v in VSB:
                p.memset("pool", v[:], 1.0, W=[v])
            ngroups = 32 if stage >= 1 else 1
            ntile = ngroups * 4
            UTB = [[Buf('utb%d_%d' % (a_, b_)) for b_ in range(4)] for a_ in range(2)]

            hds = {}

            def partA1(T):
                hds[T] = norm_tile.a1(x_all[T * 128:(T + 1) * 128, :], [], GM, SHM)

            def partA2(T):
                g, t = divmod(T, 4)
                norm_tile.a2(hds.pop(T), UT[g % 2], t * 128, UTB[g % 2][t])

            def partB(T):
                g, t = divmod(T, 4)
                uT = UT[g % 2]
                ub = UTB[g % 2][t]
                uball = UTB[g % 2]
                vsb = VSB[g % 2]
                rot["psa"] += 1
                pv = PSA[rot["psa"] % 5]
                for kc in range(8):
                    p.mm(pv[:], uT[:, kc, t * 128:(t + 1) * 128], wkv[:, kc, 512:1024], start=(kc == 0), stop=(kc == 7),
                         R=[ub, wkv], W=[pv])
                p.cp("act", vsb[:, :, t, 0:64], pv[:].rearrange("p (h d) -> p h d", h=8), R=[pv], W=[vsb])
                for kc in range(8):
                    p.mm(psF[:, t * 8:(t + 1) * 8], uT[:, kc, t * 128:(t + 1) * 128], wkv[:, kc, 1024:1032],
                         start=(kc == 0), stop=(kc == 7), R=[ub, wkv], W=[psF])
                if t < 3:
                    return
                for cj in range(4):
                    rot["psa"] += 1
                    pk = PSA[rot["psa"] % 5]
                    for kc in range(8):
                        p.mm(pk[:], wkv[:, kc, cj * 128:(cj + 1) * 128], uT[:, kc, :], start=(kc == 0), stop=(kc == 7),
                             R=uball + [wkv], W=[pk])
                    ksb = KSB[cj % 2]
                    p.cp("act", ksb[:], pk[:], R=[pk], W=[ksb])
                    for hh in range(2):
                        p.dma("act", Ks[2 * cj + hh, :, g * 512:(g + 1) * 512], ksb[hh * 64:(hh + 1) * 64, :], "st",
                              R=[ksb], W=[dKs])
                p.dma("act", Vs[:, :, 4 * g:4 * g + 4, :].rearrange("h p b c -> p h b c"), vsb[:], "st", R=[vsb], W=[dVs])
                p.tt("dve", fa[:], psF[:, 0:32], bfb[:].rearrange("p a b -> p (a b)"), ALU.add, R=[psF, bfb], W=[fa])
                p.stt("dve", fb_[:], fa[:], -1.0, fa[:], ALU.mult, ALU.max, R=[fa], W=[fb_])
                p.act(fc[:], fb_[:], AF.Exp, R=[fb_], W=[fc], scale=-1.0)
                p.act(fc[:], fc[:], AF.Ln, R=[fc, onec], W=[fc], bias=onec[:])
                p.ts("dve", fd[:], fa[:], 0.0, None, ALU.min, R=[fa], W=[fd])
                p.tt("dve", LF[:, 4 * g:4 * g + 4, :], fd[:].rearrange("p (a b) -> p a b", a=4),
                     fc[:].rearrange("p (a b) -> p a b", a=4), ALU.subtract, R=[fd, fc], W=[LF])

            partA1(0)
            if ntile > 1:
                partA1(1)
            partA2(0)
            for T in range(ntile):
                if T + 2 < ntile:
                    partA1(T + 2)
                if T + 1 < ntile:
                    partA2(T + 1)
                partB(T)
            CI = sb("CI", [128, 1024], F32, ph)
            TOTa = sb("TOTa", [128, 128, 8], F32, ph)
            TOTb = sb("TOTb", [128, 128, 8], F32, ph)
            TOT0 = sb("TOT0", [128, 128, 8], F32, ph)
            CUM = sb("CUM", [128, 128, 8], F32, ph)
            R1 = sb("R1", [128, 1024], F32, ph)
            NN = sb("NN", [128, 128, 24], BF16, ph)
            ckT = sb("ckT", [24, S], BF16, ph)
            trif = sb("trif", [128, 128], F32, ph)
            p.dma("sp", trif[:], tri, "ld", W=[trif])
            LFf = LF[:].rearrange("p a b -> p (a b)")
            for hf in range(2):
                pa = psa()
                p.mm(pa[:], trif[:], LFf[:, hf * 512:(hf + 1) * 512], R=[trif, LF], W=[pa])
                p.cp("act", CI[:, hf * 512:(hf + 1) * 512], pa[:], R=[pa], W=[CI])
                pb = psa()
                p.mm(pb[:], onesf[:], LFf[:, hf * 512:(hf + 1) * 512], R=[onesf, LF], W=[pb])
                p.cp("act", TOT0[:].rearrange("p a b -> p (a b)")[:, hf * 512:(hf + 1) * 512], pb[:], R=[pb], W=[TOT0])
            p.cp("dve", TOTa[:], TOT0[:], R=[TOT0], W=[TOTa])
            a, b = TOTa, TOTb
            s = 1
            while s < 128:
                p.tt("dve", b[:, s:, :], a[:, s:, :], a[:, :128 - s, :], ALU.add, R=[a], W=[b])
                p.cp("dve", b[:, :s, :], a[:, :s, :], R=[a], W=[b])
                a, b = b, a
                s *= 2
            p.tt("dve", b[:], a[:], TOT0[:], ALU.subtract, R=[a, TOT0], W=[b])
            p.tt("dve", CUM[:], CI[:].rearrange("p (a b) -> p a b", b=8), b[:], ALU.add, R=[CI, b], W=[CUM])

            def split3(src3, neg, dstNN, nb):
                sgn = -1.0 if neg else 1.0
                r1 = R1[:, 0:nb * 8].rearrange("p (a b) -> p a b", b=8)
                p.ts("dve", dstNN[:, :, 0:8], src3, sgn, None, ALU.mult, R=[CUM, OCt], W=[dstNN])
                p.stt("dve", r1, src3, sgn, dstNN[:, :, 0:8], ALU.mult, ALU.subtract, R=[CUM, OCt, dstNN], W=[R1])
                p.cp("dve", dstNN[:, :, 8:16], r1, R=[R1], W=[dstNN])
                p.tt("dve", r1, r1, dstNN[:, :, 8:16], ALU.subtract, R=[R1, dstNN], W=[R1])
                p.cp("dve", dstNN[:, :, 16:24], r1, R=[R1], W=[dstNN])

            OCt = sb("OCt", [128, 16, 8], F32, ph)
            QN = sb("QN", [128, 16, 24], BF16, ph)
            cqT = sb("cqT", [24, 2048], BF16, ph)
            split3(CUM[:], True, NN, 128)
            for b8 in range(16):
                pt = pst()
                for j in range(8):
                    blk = b8 * 8 + j
                    p.tr(pt[0:24, j * 128:(j + 1) * 128], NN[:, blk, :], idb[:], R=[NN, idb], W=[pt])
                p.cp("act", ckT[:, b8 * 1024:(b8 + 1) * 1024], pt[0:24, :], R=[pt], W=[ckT])
            p.dma("sp", Kc, ckT[:], "st", R=[ckT], W=[dKc])
            CUM4 = CUM[:].rearrange("p (m r) h -> p m r h", r=8)
            p.ts("dve", OCt[:], CUM4[:, :, 0, :], metat[:, 0:1], None, ALU.mult, R=[CUM, metat], W=[OCt])
            for r in range(1, 8):
                p.stt("dve", OCt[:], CUM4[:, :, r, :], metat[:, r:r + 1], OCt[:], ALU.mult, ALU.add, R=[CUM, metat, OCt], W=[OCt])
            split3(OCt[:], False, QN, 16)
            for b8 in range(2):
                pt = pst()
                for j in range(8):
                    p.tr(pt[0:24, j * 128:(j + 1) * 128], QN[:, b8 * 8 + j, :], idb[:], R=[QN, idb], W=[pt])
                p.cp("act", cqT[:, b8 * 1024:(b8 + 1) * 1024], pt[0:24, :], R=[pt], W=[cqT])
            p.dma("sp", Qc, cqT[:], "st", R=[cqT], W=[dQc])
            p.flush()

        with ExitStack() as ph:
            norm_tile = mk_norm(ph, "c")
            wq = sb("wq", [128, 8, 1280], BF16, ph)
            UTo = sb("uTo", [128, 8, 512], BF16, ph)
            UTh = sb("uTh", [128, 8, 512], BF16, ph)
            QSB = [sb("qsb%d" % i, [128, 512], BF16, ph) for i in range(2)]
            QA = sb("QA", [64, 8, 512], BF16, ph)
            KA = sb("KA", [64, 2, 2, 512], BF16, ph)
            VA = sb("VA", [128, 2, 4, 2, 65], BF16, ph)
            BM = sb("BM", [128, 2, 8, 128], F32, ph)
            MK = sb("MK", [128, 2, 128], F32, ph)
            SBI = [sb("sbi%d" % i, [128, 512], F32, ph) for i in range(2)]
            PTs = [sb("pts%d" % i, [128, 512], BF16, ph) for i in range(2)]
            den = sb("den", [128, 8], F32, ph)
            yat = sb("yat", [128, 512], BF16, ph)
            YAT = sb("YATt", [128, 4, 128], BF16, ph)
            p.dma("pool", wq[:], w_in[:, 0:1280].rearrange("(k p) n -> p k n", p=128), "cast", W=[wq])
            p.dma("sp", BM[:].rearrange("p a h q -> p (a h q)"), swa_bias, "ld", W=[BM])
            p.dma("sp", MK[:].rearrange("p a q -> p (a q)"), swa_mask, "ld", W=[MK])
            for hf in range(2):
                for h in range(8):
                    p.tt("dve", BM[:, hf, h, :], BM[:, hf, h, :], MK[:, hf, :], ALU.add, R=[BM, MK], W=[BM])
            p.memset("pool", VA[:], 1.0, W=[VA])
            for gq in range(4 if stage >= 2 else 0):
                for t in range(4):
                    m = 4 * gq + t
                    norm_tile(x_own[m * 128:(m + 1) * 128, :], [], GM, SHM, UTo, t * 128)
                    norm_tile(x_halo[m * 128:(m + 1) * 128, :], [], GM, SHM, UTh, t * 128)
                for cj in range(4):
                    pq = psa()
                    for kc in range(8):
                        p.mm(pq[:], wq[:, kc, 768 + cj * 128:768 + (cj + 1) * 128], UTo[:, kc, :], start=(kc == 0), stop=(kc == 7),
                             R=[wq, UTo], W=[pq])
                    qsb = QSB[cj % 2]
                    p.act(qsb[:], pq[:], AF.Copy, R=[pq], W=[qsb], scale=0.125)
                    for hh in range(2):
                        p.dma("sp", Qs[2 * cj + hh, :, gq * 512:(gq + 1) * 512], qsb[hh * 64:(hh + 1) * 64, :], "st",
                              R=[qsb], W=[dQs])
                for h in range(8):
                    pq = psa()
                    for kc in range(8):
                        p.mm(pq[0:64, :], wq[:, kc, h * 64:(h + 1) * 64], UTo[:, kc, :], start=(kc == 0), stop=(kc == 7),
                             R=[wq, UTo], W=[pq])
                    p.act(QA[:, h, :], pq[0:64, :], AF.Copy, R=[pq], W=[QA], scale=0.125)
                for si, UTs in enumerate((UTh, UTo)):
                    for kvh in range(2):
                        pq = psa()
                        for kc in range(8):
                            p.mm(pq[0:64, :], wq[:, kc, 512 + kvh * 64:512 + (kvh + 1) * 64], UTs[:, kc, :],
                                 start=(kc == 0), stop=(kc == 7), R=[wq, UTs], W=[pq])
                        p.cp("act", KA[:, si, kvh, :], pq[0:64, :], R=[pq], W=[KA])
                    for t in range(4):
                        pq = psa()
                        for kc in range(8):
                            p.mm(pq[:, 0:128], UTs[:, kc, t * 128:(t + 1) * 128], wq[:, kc, 640:768],
                                 start=(kc == 0), stop=(kc == 7), R=[wq, UTs], W=[pq])
                        p.cp("dve", VA[:, si, t, :, 0:64], pq[:, 0:128].rearrange("p (a d) -> p a d", a=2), R=[pq], W=[VA])
                for t in range(4):
                    m = 4 * gq + t
                    pos = [psa(), psa()]
                    for kvh in range(2):
                        po = pos[kvh]
                        pTs = []
                        for si in range(2):
                            pS = psa()
                            p.mm(pS[:], KA[:, si, kvh, t * 128:(t + 1) * 128], QA[:, kvh * 4:(kvh + 1) * 4, t * 128:(t + 1) * 128],
                                 R=[KA, QA], W=[pS])
                            sbi = SBI[si]
                            p.tt("dve", sbi[:], pS[:], BM[:, si, kvh * 4:(kvh + 1) * 4, :].rearrange("p h q -> p (h q)"),
                                 ALU.add, R=[pS, BM], W=[sbi])
                            pT = PTs[si]
                            if si == 0:
                                p.act(pT[:], sbi[:], AF.Exp, R=[sbi, metat], W=[pT], bias=metat[:, 8 + m:9 + m])
                            else:
                                p.act(pT[:], sbi[:], AF.Exp, R=[sbi], W=[pT])
                            pTs.append(pT)
                        for g4 in range(4):
                            for si in range(2):
                                p.mm(po[:, g4 * 65:(g4 + 1) * 65], pTs[si][:, g4 * 128:(g4 + 1) * 128], VA[:, si, t, kvh, :],
                                     start=(si == 0), stop=(si == 1), R=[pTs[si], VA], W=[po])
                    for kvh in range(2):
                        po = pos[kvh]
                        po3 = po[:, 0:260].rearrange("p (g c) -> p g c", c=65)
                        p.tt("dve", den[:, kvh * 4:(kvh + 1) * 4], po3[:, :, 64], esink[:, kvh * 4:(kvh + 1) * 4], ALU.add,
                             R=[po, esink], W=[den])
                        p.op("dve", lambda e, k=kvh: e.reciprocal(den[:, k * 4:(k + 1) * 4], den[:, k * 4:(k + 1) * 4]), R=[den], W=[den])
                        for g4 in range(4):
                            h = kvh * 4 + g4
                            p.ts("dve", yat[:, h * 64:(h + 1) * 64], po[:, g4 * 65:g4 * 65 + 64], den[:, h:h + 1], None, ALU.mult,
                                 R=[po, den], W=[yat])
                    pt = pst()
                    for c4 in range(4):
                        p.tr(pt[:, c4 * 128:(c4 + 1) * 128], yat[:, c4 * 128:(c4 + 1) * 128], idb[:], R=[yat, idb], W=[pt])
                    p.cp("act", YAT[:], pt[:, 0:512].rearrange("p (c t) -> p c t", c=4), R=[pt], W=[YAT])
                    p.dma("sp", YA[:, m * 128:(m + 1) * 128].rearrange("(c p) t -> p c t", p=128), YAT[:], "st", R=[YAT], W=[dYA])
            p.flush()

        with ExitStack() as ph:
            KAUG = [sb("kaug%d" % i, [70, S], BF16, ph) for i in range(2)]
            VH = [sb("vh%d" % i, [128, 128, 65], BF16, ph) for i in range(2)]
            QH = [sb("qh%d" % i, [70, 2048], BF16, ph) for i in range(2)]
            PT = [sb("pt%d" % i, [128, 512], BF16, ph) for i in range(3)]
            MSK = [sb("msk%d" % i, [128, 128], F32, ph) for i in range(2)]
            FM = sb("FM", [128, 8, 128], F32, ph)
            LR = sb("LR", [65, 512], F32, ph)
            BCS = sb("BCS", [64, 512], F32, ph)
            YBS = [sb("ybs%d" % i, [64, 512], BF16, ph) for i in range(2)]
            p.dma("sp", FM[:].rearrange("p a q -> p (a q)"), fmask, "ld", W=[FM])
            for i in range(2):
                p.memset("pool", KAUG[i][64:70, :], 1.0, W=[KAUG[i]])
                p.memset("pool", QH[i][64:70, :], 1.0, W=[QH[i]])
            Kc3 = Kc.rearrange("(x h) t -> x h t", x=3)
            Qc3 = Qc.rearrange("(x h) t -> x h t", x=3)
            PSS = PSA[0:3]
            PSO = PSA[3:5]
            psBC = PSA[5]
            its = []
            gi = 0
            for h in range(8 if stage >= 3 else 0):
                for g in range(4):
                    q0 = g * 512
                    lst = []
                    for kb in range(32 * g):
                        lst.append(dict(h=h, kb=kb, n=512, c0=0, q0=q0, masked=False, kb8=0, gi=gi))
                    for r in range(4):
                        for kb8 in range(8):
                            lst.append(dict(h=h, kb=32 * g + 8 * r + kb8, n=(4 - r) * 128, c0=r * 128, q0=q0, masked=True,
                                            kb8=kb8, gi=gi))
                    lst[0]["first"] = True
                    lst[-1]["last"] = True
                    its.extend(lst)
                    gi += 1
            loaded = set()

            def load_head(h):
                if h in loaded or h >= 8:
                    return
                loaded.add(h)
                ka, vh, qh = KAUG[h % 2], VH[h % 2], QH[h % 2]
                p.dma("sp", ka[0:64, :], Ks[h], "ldk", R=[dKs], W=[ka])
                p.dma("sp", ka[67:70, :], Kc3[:, h, :], "ldk", R=[dKc], W=[ka])
                p.dma("sp", vh[:], Vs[h], "ldk", R=[dVs], W=[vh])
                p.dma("sp", qh[0:64, :], Qs[h], "ldk", R=[dQs], W=[qh])
                p.dma("sp", qh[64:67, :], Qc3[:, h, :], "ldk", R=[dQc], W=[qh])

            def emitS(i):
                d = its[i]
                h = d["h"]
                load_head(h)
                ka, qh = KAUG[h % 2], QH[h % 2]
                pS, pT = PSS[i % 3], PT[i % 3]
                n, c0, q0, kb = d["n"], d["c0"], d["q0"], d["kb"]
                p.mm(pS[:, 0:n], ka[0:70, kb * 128:(kb + 1) * 128], qh[0:70, q0 + c0:q0 + 512], R=[ka, qh], W=[pS])
                if d["masked"]:
                    mk = MSK[i % 2]
                    p.tt("dve", mk[:], pS[:, 0:128], FM[:, d["kb8"], :], ALU.min, R=[pS, FM], W=[mk])
                    p.act(pT[:, 0:128], mk[:], AF.Exp, R=[mk], W=[pT])
                    if n > 128:
                        p.act(pT[:, 128:n], pS[:, 128:n], AF.Exp, R=[pS], W=[pT])
                else:
                    p.act(pT[:], pS[:], AF.Exp, R=[pS], W=[pT])

            def emitPV(i):
                d = its[i]
                h = d["h"]
                vh = VH[h % 2]
                po = PSO[d["gi"] % 2]
                pT = PT[i % 3]
                n, c0, q0, kb = d["n"], d["c0"], d["q0"], d["kb"]
                p.mm(po[0:65, c0:512], vh[:, kb, :], pT[:, 0:n], start=bool(d.get("first")), stop=bool(d.get("last")),
                     R=[vh, pT], W=[po])
                if d.get("last"):
                    if q0 == 0:
                        load_head(h + 1)
                    p.cp("act", LR[64:65, :], po[64:65, :], R=[po], W=[LR])
                    p.op("dve", lambda e: e.reciprocal(LR[64:65, :], LR[64:65, :]), R=[LR], W=[LR])
                    p.mm(psBC[0:64, :], onesf[64:65, 0:64], LR[64:65, :], R=[onesf, LR], W=[psBC])
                    p.cp("act", BCS[:], psBC[0:64, :], R=[psBC], W=[BCS])
                    ybs = YBS[d["gi"] % 2]
                    p.tt("dve", ybs[:], po[0:64, :], BCS[:], ALU.mult, R=[po, BCS], W=[ybs])
                    p.dma("sp", YB[h, :, q0:q0 + 512], ybs[:], "st", R=[ybs], W=[dYB])

            LA = 2
            for i in range(len(its) + LA):
                if i < len(its):
                    emitS(i)
                if i - LA >= 0:
                    emitPV(i - LA)
            p.flush()

        with ExitStack() as ph:
            norm_tile = mk_norm(ph, "e")
            wg = sb("wg", [128, 8, 2048], BF16, ph)
            wpa = sb("wpa", [128, 4, 1024], BF16, ph)
            wpb = sb("wpb", [64, 8, 1024], BF16, ph)
            wo = sb("wo", [128, 8, 1024], BF16, ph)
            UTb = sb("uTb", [128, 8, 128], BF16, ph)
            SG = sb("SG", [128, 2048], F32, ph)
            yaTb = sb("yaTb", [128, 4, 128], BF16, ph)
            ybTb = sb("ybTb", [64, 8, 128], BF16, ph)
            T1 = sb("T1", [128, 512], F32, ph)
            T2 = sb("T2", [128, 512], F32, ph)
            MG = sb("MG", [128, 1024], BF16, ph)
            MGT = sb("MGT", [128, 8, 128], BF16, ph)
            T3 = sb("T3", [128, 1024], F32, ph)
            X1t = [sb("x1t%d" % i, [128, 1024], F32, ph) for i in range(2)]
            p.dma("pool", wg[:], w_in[:, 2312:4360].rearrange("(k p) n -> p k n", p=128), "cast", W=[wg])
            p.dma("pool", wpa[:], w_pa.rearrange("(c p) n -> p c n", p=128), "cast", W=[wpa])
            p.dma("pool", wpb[:], w_pb.rearrange("(h p) n -> p h n", p=64), "cast", W=[wpb])
            p.dma("pool", wo[:], w_out.rearrange("(k p) n -> p k n", p=128), "cast", W=[wo])
            ZT = sb("ZT", [128, 4, 1024], BF16, ph)
            p.memset("pool", ZT[:], 0.0, W=[ZT])
            for u_ in range(48):
                p.dma("sp", Xs[u_ * 512:(u_ + 1) * 512, :].rearrange("(a p) n -> p a n", p=128), ZT[:], "st", R=[ZT], W=[dXs])
            UTbs = [UTb, sb("uTb2", [128, 8, 128], BF16, ph)]
            SGs_ = [SG, sb("SG2", [128, 2048], F32, ph)]
            yaTbs = [yaTb, sb("yaTb2", [128, 4, 128], BF16, ph)]
            ybTbs = [ybTb, sb("ybTb2", [64, 8, 128], BF16, ph)]
            xts = {}

            def stage1(m):
                UTb_, SG_, ya_, yb_ = UTbs[m % 2], SGs_[m % 2], yaTbs[m % 2], ybTbs[m % 2]
                xts[m] = norm_tile(x_own[m * 128:(m + 1) * 128, :], [], GM, SHM, UTb_, 0)
                p.dma("sp", ya_[:], YA[:, m * 128:(m + 1) * 128].rearrange("(c p) t -> p c t", p=128), "ldy", R=[dYA], W=[ya_])
                p.dma("sp", yb_[:], YB[:, :, m * 128:(m + 1) * 128].rearrange("h d t -> d h t"), "ldy", R=[dYB], W=[yb_])
                for j in range(4):
                    pg = psa()
                    for kc in range(8):
                        p.mm(pg[:], UTb_[:, kc, :], wg[:, kc, j * 512:(j + 1) * 512], start=(kc == 0), stop=(kc == 7),
                             R=[UTb_, wg], W=[pg])
                    p.act(SG_[:, j * 512:(j + 1) * 512], pg[:], AF.Sigmoid, R=[pg], W=[SG_])

            def stage2(m):
                SG_, ya_, yb_ = SGs_[m % 2], yaTbs[m % 2], ybTbs[m % 2]
                xt = xts.pop(m)
                for hf in range(2):
                    pa = psa()
                    for c4 in range(4):
                        p.mm(pa[:], ya_[:, c4, :], wpa[:, c4, hf * 512:(hf + 1) * 512], start=(c4 == 0), stop=(c4 == 3),
                             R=[ya_, wpa], W=[pa])
                    pb = psa()
                    for h in range(8):
                        p.mm(pb[:], yb_[:, h, :], wpb[:, h, hf * 512:(hf + 1) * 512], start=(h == 0), stop=(h == 7),
                             R=[yb_, wpb], W=[pb])
                    p.tt("dve", T1[:], pa[:], SG_[:, hf * 512:(hf + 1) * 512], ALU.mult, R=[pa, SG_], W=[T1])
                    p.tt("dve", T2[:], pb[:], SG_[:, 1024 + hf * 512:1024 + (hf + 1) * 512], ALU.mult, R=[pb, SG_], W=[T2])
                    p.tt("pool", MG[:, hf * 512:(hf + 1) * 512], T1[:], T2[:], ALU.add, R=[T1, T2], W=[MG])
                pt = pst()
                for kc in range(8):
                    p.tr(pt[:, kc * 128:(kc + 1) * 128], MG[:, kc * 128:(kc + 1) * 128], idb[:], R=[MG, idb], W=[pt])
                p.cp("act", MGT[:], pt[:].rearrange("p (k t) -> p k t", k=8), R=[pt], W=[MGT])
                x1 = X1t[m % 2]
                for hf in range(2):
                    pw = psa()
                    for kc in range(8):
                        p.mm(pw[:], MGT[:, kc, :], wo[:, kc, hf * 512:(hf + 1) * 512], start=(kc == 0), stop=(kc == 7),
                             R=[MGT, wo], W=[pw])
                    p.tt("dve", T3[:, hf * 512:(hf + 1) * 512], pw[:], GTM[:, hf * 512:(hf + 1) * 512], ALU.mult, R=[pw, GTM], W=[T3])
                p.tt("pool", x1[:], T3[:], xt[:], ALU.add, R=[T3, xt], W=[x1])
                p.dma("sp", X1[m * 128:(m + 1) * 128, :], x1[:], "st", R=[x1], W=[dX1])

            nblk = 16 if stage >= 4 else 0
            if nblk:
                stage1(0)
            for m in range(nblk):
                if m + 1 < nblk:
                    stage1(m + 1)
                stage2(m)
            p.flush()

        esm.close()
        NU = 48
        NSLOT = NU * 512
        with ExitStack() as phF:
            CWF = sb("CWF", [128, 16, 32], F32, phF)
            CWFB = [Buf("cwf%d" % i) for i in range(16)]
            CW4 = sb("CW4", [128, 16, 4], F32, phF)
            SLI = sb("SLI", [128, 64], I32, phF)
            EU = sb("EU", [128, NU], F32, phF)
            EU1024 = sb("EU1024", [128, NU], F32, phF)
            EU128 = sb("EU128", [128, NU], F32, phF)
            mc = sb("mc", [128, NU + 9], F32, phF)
            b2s = sb("b2s", [32, 1024], F32, phF)
            p.dma("sp", mc[:], mconst, "ld", W=[mc])
            p.dma("sp", b2s[:], b2, "ld", W=[b2s])
            uvals = mc[:, 0:NU]
            rowoff = mc[:, NU:NU + 8]
            pidx = mc[:, NU + 8:NU + 9]
            with ExitStack() as ph:
                norm_tile = mk_norm(ph, "f", 2, 1, 1)
                U2 = [sb("U2_%d" % i, [128, 1024], BF16, ph) for i in range(16)]
                U2Tt = [sb("U2Tt%d" % i, [128, 8, 128], BF16, ph) for i in range(2)]
                wr = sb("wr", [128, 8, 32], BF16, ph)
                trs = sb("trs", [128, 128], F32, ph)
                LGs = sb("LGs", [128, 16, 32], F32, ph)
                M8s = sb("M8s", [128, 16, 8], F32, ph)
                MK = sb("MKrt", [128, 16, 32], F32, ph)
                NM = sb("NM", [128, 1], F32, ph)
                EX4 = sb("EX4", [128, 4], F32, ph)
                S4 = sb("S4", [128, 1], F32, ph)
                CNT = sb("CNT", [128, 32], F32, ph)
                PADc = sb("PADc", [128, 32], F32, ph)
                TMPc = sb("TMPc", [128, 32], F32, ph)
                ENDa = sb("ENDa", [128, 32], F32, ph)
                ENDb = sb("ENDb", [128, 32], F32, ph)
                BASE = sb("BASE", [128, 32], F32, ph)
                TU = sb("TU", [128, NU], F32, ph)
                SLT = sb("SLT", [128, 32], F32, ph)
                OH = sb("OH", [128, 32], F32, ph)
                OHS = sb("OHS", [128, 32], F32, ph)
                SLF = sb("SLF", [128, 64], F32, ph)
                SLFB = [Buf("slf%d" % i) for i in range(64)]
                OHr = [sb("OHr%d" % i, [128, 32], F32, ph) for i in range(8)]
                OSr = [sb("OSr%d" % i, [128, 32], F32, ph) for i in range(8)]
                p.dma("pool", wr[:], w_router.rearrange("(k p) n -> p k n", p=128), "cast", W=[wr])
                p.dma("sp", trs[:], tris, "ld", W=[trs])

                for t in range(16):
                    hd = norm_tile.a1(X1[t * 128:(t + 1) * 128, :], [dX1], GF, SHF)
                    xt_, u_ = hd
                    p.cp("pool", U2[t][:, :], u_[:], R=[u_], W=[U2[t]])
                    ut = U2Tt[t % 2]
                    norm_tile.a2(hd, ut, 0)
                    pr = psa()
                    for kc in range(8):
                        p.mm(pr[:, 0:32], ut[:, kc, :], wr[:, kc, :], start=(kc == 0), stop=(kc == 7), R=[ut, wr], W=[pr])
                    p.tt("dve", LGs[:, t, :], pr[:, 0:32], brb[:], ALU.add, R=[pr, brb], W=[LGs])
                    p.op("dve", lambda e, t=t: e.max(M8s[:, t, :], LGs[:, t, :]), R=[LGs], W=[M8s])
                    p.ts("dve", MK[:, t, :], LGs[:, t, :], M8s[:, t, 3:4], None, ALU.is_ge, R=[LGs, M8s], W=[MK])
                    p.ts("dve", NM[:], M8s[:, t, 0:1], -1.0, None, ALU.mult, R=[M8s], W=[NM])
                    p.act(EX4[:], M8s[:, t, 0:4], AF.Exp, R=[M8s, NM], W=[EX4], bias=NM[:])
                    p.op("dve", lambda e: e.tensor_reduce(S4[:], EX4[:], AX.X, ALU.add), R=[EX4], W=[S4])
                    p.op("dve", lambda e: e.reciprocal(S4[:], S4[:]), R=[S4], W=[S4])
                    p.ts("dve", CW4[:, t, :], EX4[:], S4[:], None, ALU.mult, R=[EX4, S4], W=[CW4])
                pcn = psa()
                for t in range(16):
                    p.mm(pcn[:, 0:32], onesf[:], MK[:, t, :], start=(t == 0), stop=(t == 15), R=[onesf, MK], W=[pcn])
                p.cp("dve", CNT[:], pcn[:, 0:32], R=[pcn], W=[CNT])
                p.ts("dve", PADc[:], CNT[:], 0.0, None, ALU.is_gt, R=[CNT], W=[PADc])
                for thr in (512.0, 1024.0, 1536.0):
                    p.ts("dve", TMPc[:], CNT[:], thr, None, ALU.is_gt, R=[CNT], W=[TMPc])
                    p.tt("dve", PADc[:], PADc[:], TMPc[:], ALU.add, R=[PADc, TMPc], W=[PADc])
                p.ts("dve", PADc[:], PADc[:], 512.0, None, ALU.mult, R=[PADc], W=[PADc])
                p.cp("dve", ENDa[:], PADc[:], R=[PADc], W=[ENDa])
                a_, b_ = ENDa, ENDb
                sft = 1
                while sft < 32:
                    p.tt("dve", b_[:, sft:], a_[:, sft:], a_[:, :32 - sft], ALU.add, R=[a_], W=[b_])
                    p.cp("dve", b_[:, :sft], a_[:, :sft], R=[a_], W=[b_])
                    a_, b_ = b_, a_
                    sft *= 2
                END = a_
                p.tt("dve", BASE[:], END[:], PADc[:], ALU.subtract, R=[END, PADc], W=[BASE])
                p.memset("dve", EU[:], 0.0, W=[EU])
                for e_ in range(32):
                    p.ts("dve", TU[:], uvals, END[:, e_:e_ + 1], None, ALU.is_ge, R=[mc, END], W=[TU])
                    p.tt("dve", EU[:], EU[:], TU[:], ALU.add, R=[EU, TU], W=[EU])
                p.ts("dve", EU1024[:], EU[:], 1024.0, None, ALU.mult, R=[EU], W=[EU1024])
                p.ts("dve", EU128[:], EU[:], 128.0, None, ALU.mult, R=[EU], W=[EU128])
                p.memset("dve", CWF[:], 0.0, W=CWFB)
                for t in range(16):
                    pp = psa()
                    for j in range(t):
                        p.mm(pp[:, 0:32], onesf[:], MK[:, j, :], start=(j == 0), stop=False, R=[onesf, MK], W=[pp])
                    p.mm(pp[:, 0:32], trs[:], MK[:, t, :], start=(t == 0), stop=True, R=[trs, MK], W=[pp])
                    p.tt("dve", SLT[:], pp[:, 0:32], BASE[:], ALU.add, R=[pp, BASE], W=[SLT])
                    ohs_ = [OHr[(4 * t + k) % 8] for k in range(4)]
                    oss_ = [OSr[(4 * t + k) % 8] for k in range(4)]
                    for k in range(4):
                        p.ts("dve", ohs_[k][:], LGs[:, t, :], M8s[:, t, k:k + 1], None, ALU.is_equal, R=[LGs, M8s], W=[ohs_[k]])
                    for k in range(4):
                        p.tt("dve", oss_[k][:], ohs_[k][:], SLT[:], ALU.mult, R=[ohs_[k], SLT], W=[oss_[k]])
                    for k in range(4):
                        p.op("dve", lambda e, j=4 * t + k, o_=oss_[k]: e.tensor_reduce(SLF[:, j:j + 1], o_[:], AX.X, ALU.add),
                             R=[oss_[k]], W=[SLFB[4 * t + k]])
                    for k in range(4):
                        p.stt("dve", CWF[:, t, :], ohs_[k][:], CW4[:, t, k:k + 1], CWF[:, t, :], ALU.mult, ALU.add,
                              R=[ohs_[k], CW4, CWFB[t]], W=[CWFB[t]])
                p.ts("dve", SLI[:], SLF[:], 0.0, None, ALU.add, R=SLFB, W=[SLI])
                if DEBUG:
                    p.dma("sp", DBG[:, 0:64], SLF[:], "st", R=SLFB)
                    p.dma("sp", DBG[:, 64:112], EU[:], "st", R=[EU])
                    p.dma("sp", DBG[:, 112:144], CNT[:], "st", R=[CNT])
                    p.dma("sp", DBG[:, 144:176], END[:], "st", R=[END])
                    p.dma("sp", DBG[:, 176:240], CW4[:].rearrange("p a b -> p (a b)"), "st", R=[CW4])
                for t in range(16):
                    for k in range(4):
                        j = 4 * t + k
                        p.op("pool", lambda e, t=t, j=j: e.indirect_dma_start(
                            out=Xs, out_offset=bass.IndirectOffsetOnAxis(ap=SLI[:, j:j + 1], axis=0),
                            in_=U2[t][:, :], in_offset=None),
                            R=[U2[t], SLI], W=[dXs], dma="ind")
                p.flush()

            with ExitStack() as ph:
                W1G = [[sb("w1g%d_%d" % (i, k_), [128, 1024], BF16, ph) for k_ in range(8)] for i in range(2)]
                W1L = [[sb("w1l%d_%d" % (i, k_), [128, 1024], BF16, ph) for k_ in range(8)] for i in range(2)]
                W2 = [[sb("w2_%d_%d" % (i, k_), [128, 1024], BF16, ph) for k_ in range(8)] for i in range(2)]
                XS = [sb("xs%d" % i, [128, 4, 1024], BF16, ph) for i in range(2)]
                XTu = [sb("xtu%d" % i, [128, 8, 512], BF16, ph) for i in range(2)]
                ATs = [sb("AT%d" % i, [128, 8, 512], BF16, ph) for i in range(2)]
                IDX = [sb("idx%d" % i, [128, 8], I32, ph) for i in range(3)]
                IDXB = [sb("idxb%d" % i, [128, 1], I32, ph) for i in range(3)]
                B1G = [sb("b1gu%d" % i, [128, 8], F32, ph) for i in range(3)]
                B1L = [sb("b1lu%d" % i, [128, 8], F32, ph) for i in range(3)]
                GLs = [sb("GLt%d" % i, [128, 512], F32, ph) for i in range(2)]
                SGs = [sb("SGt%d" % i, [128, 512], F32, ph) for i in range(2)]
                HBs = [sb("HBt%d" % i, [128, 512], F32, ph) for i in range(2)]
                L2s = [sb("L2t%d" % i, [128, 512], F32, ph) for i in range(2)]
                YO = [sb("yo%d" % i, [128, 1024], F32, ph) for i in range(2)]
                w1g_rows = w1g.rearrange("e k n -> (e k) n")
                w1l_rows = w1l.rearrange("e k n -> (e k) n")
                w2_rows = w2.rearrange("e k n -> (e k) n")
                cc = [0]
                yc = [0]

                bregs = {}

                def gather(dst_ap, src_rows, idx_ap, nrows, R, W):
                    def fn(e):
                        if nrows not in bregs:
                            bregs[nrows] = e.to_reg(nrows - 1)
                        return e.indirect_dma_start(
                            out=dst_ap, out_offset=None, in_=src_rows,
                            in_offset=bass.IndirectOffsetOnAxis(ap=idx_ap, axis=0),
                            bounds_check=bregs[nrows], oob_is_err=False)
                    p.op("pool", fn, R=R, W=W, dma="ind")

                ORD = []
                for g_ in range(12):
                    ORD += [3 * g_, 3 * g_ + 1, 3 * g_ + 2, 47 - g_]
                assert sorted(ORD) == list(range(NU))

                def prep_w1(u):
                    if u >= NU:
                        return
                    uid = ORD[u]
                    ix, ixb = IDX[u % 3], IDXB[u % 3]
                    p.ts("dve", ix[:], rowoff, EU1024[:, uid:uid + 1], None, ALU.add, R=[mc, EU1024], W=[ix])
                    p.ts("dve", ixb[:], pidx, EU128[:, uid:uid + 1], None, ALU.add, R=[mc, EU128], W=[ixb])
                    for kc in range(8):
                        gather(W1G[u % 2][kc][:, :], w1g_rows, ix[:, kc:kc + 1], 32768, [ix], [W1G[u % 2][kc]])
                    for kc in range(8):
                        gather(W1L[u % 2][kc][:, :], w1l_rows, ix[:, kc:kc + 1], 32768, [ix], [W1L[u % 2][kc]])
                    gather(B1G[u % 3][:, :], b1g_rows, ixb[:, 0:1], 4096, [ixb], [B1G[u % 3]])
                    gather(B1L[u % 3][:, :], b1l_rows, ixb[:, 0:1], 4096, [ixb], [B1L[u % 3]])

                def prep_xs(u):
                    if u >= NU:
                        return
                    uid = ORD[u]
                    xs = XS[u % 2]
                    p.dma("sp", xs[:], Xs[uid * 512:(uid + 1) * 512, :].rearrange("(a p) n -> p a n", p=128), "ldx", R=[dXs], W=[xs])

                def prep_w2(u):
                    if u >= NU:
                        return
                    ix = IDX[u % 3]
                    for kc in range(8):
                        gather(W2[u % 2][kc][:, :], w2_rows, ix[:, kc:kc + 1], 32768, [ix], [W2[u % 2][kc]])

                def emit_mm1(u):
                    wg_, wl_ = W1G[u % 2], W1L[u % 2]
                    AT = ATs[u % 2]
                    xs, xt_ = XS[u % 2], XTu[u % 2]
                    bg, bl = B1G[u % 3], B1L[u % 3]
                    for a4 in range(4):
                        pt = pst()
                        for kc in range(8):
                            p.tr(pt[:, kc * 128:(kc + 1) * 128], xs[:, a4, kc * 128:(kc + 1) * 128], idb[:], R=[xs, idb], W=[pt])
                        p.cp("act", xt_[:, :, a4 * 128:(a4 + 1) * 128], pt[:].rearrange("p (k t) -> p k t", k=8), R=[pt], W=[xt_])
                    for c in range(8):
                        j = cc[0] % 2
                        cc[0] += 1
                        GLt, SGt, HBt, L2t = GLs[j], SGs[j], HBs[j], L2s[j]
                        pg = psa()
                        for kc in range(8):
                            p.mm(pg[:], wg_[kc][:, c * 128:(c + 1) * 128], xt_[:, kc, :],
                                 start=(kc == 0), stop=(kc == 7), R=[wg_[kc], xt_], W=[pg])
                        pl = psa()
                        for kc in range(8):
                            p.mm(pl[:], wl_[kc][:, c * 128:(c + 1) * 128], xt_[:, kc, :],
                                 start=(kc == 0), stop=(kc == 7), R=[wl_[kc], xt_], W=[pl])
                        p.ts("dve", GLt[:], pg[:], bg[:, c:c + 1], 7.0, ALU.add, ALU.min, R=[pg, bg], W=[GLt])
                        p.act(SGt[:], GLt[:], AF.Sigmoid, R=[GLt], W=[SGt], scale=1.702)
                        p.act(HBt[:], pl[:], AF.Identity, R=[pl, bl], W=[HBt], bias=bl[:, c:c + 1])
                        p.ts("dve", L2t[:], HBt[:], 7.0, -7.0, ALU.min, ALU.max, R=[HBt], W=[L2t])
                        p.tt("dve", GLt[:], GLt[:], SGt[:], ALU.mult, R=[GLt, SGt], W=[GLt])
                        p.stt("dve", AT[:, c, :], L2t[:], 1.0, GLt[:], ALU.add, ALU.mult, R=[L2t, GLt], W=[AT])

                def emit_mm2(u):
                    AT = ATs[u % 2]
                    w2_ = W2[u % 2]
                    for t4 in range(4):
                        yo = YO[yc[0] % 2]
                        yc[0] += 1
                        for hf in range(2):
                            py = psa()
                            for c in range(8):
                                p.mm(py[:], AT[:, c, t4 * 128:(t4 + 1) * 128], w2_[c][:, hf * 512:(hf + 1) * 512],
                                     start=(c == 0), stop=(c == 7), R=[AT, w2_[c]], W=[py])
                            p.cp("act", yo[:, hf * 512:(hf + 1) * 512], py[:], R=[py], W=[yo])
                        r0 = ORD[u] * 512 + t4 * 128
                        p.dma("sp", Ys[r0:r0 + 128, :], yo[:], "st", R=[yo], W=[dYs])

                nun = NU if stage >= 5 else 0
                if nun:
                    prep_xs(0)
                    prep_w1(0)
                    prep_w2(0)
                    prep_xs(1)
                    prep_w1(1)
                    emit_mm1(0)
                for u in range(nun):
                    if u + 1 < nun:
                        emit_mm1(u + 1)
                    prep_xs(u + 2)
                    emit_mm2(u)
                    prep_w1(u + 2)
                    prep_w2(u + 1)
                p.flush()

            with ExitStack() as ph:
                CWT = sb("CWTc", [32, 128], F32, ph)
                ACt = [sb("act%d" % i, [128, 1024], F32, ph) for i in range(2)]
                GB = [sb("gb%d" % i, [128, 1024], F32, ph) for i in range(8)]
                X2s = [sb("x2s%d" % i, [128, 1024], F32, ph) for i in range(2)]
                junkf = sb("junkfin", [128, 1024], BF16, ph)
                SSf = [sb("ssfin%d" % i, [128, 1], F32, ph) for i in range(2)]
                RSf = [sb("rsfin%d" % i, [128, 1], F32, ph) for i in range(2)]
                gc = [0]
                for t in range(16 if stage >= 5 else 0):
                    ac, x2, ssf, rsf = ACt[t % 2], X2s[t % 2], SSf[t % 2], RSf[t % 2]
                    p.dma("sp", x2[:], X1[t * 128:(t + 1) * 128, :], "ldx", R=[dX1], W=[x2])
                    gs = []
                    for k in range(4):
                        g_ = GB[gc[0] % 8]
                        gc[0] += 1
                        j = 4 * t + k
                        p.op("pool", lambda e, g_=g_, j=j: e.indirect_dma_start(
                            out=g_[:, :], out_offset=None, in_=Ys,
                            in_offset=bass.IndirectOffsetOnAxis(ap=SLI[:, j:j + 1], axis=0)), R=[dYs, SLI], W=[g_], dma="ind")
                        gs.append(g_)
                    pc = psa()
                    p.tr(pc[0:32, 0:128], CWF[:, t, :], idf[:], R=[CWFB[t], idf], W=[pc])
                    p.cp("act", CWT[:], pc[0:32, 0:128], R=[pc], W=[CWT])
                    for hf in range(2):
                        py = psa()
                        p.mm(py[:], CWT[:], b2s[:, hf * 512:(hf + 1) * 512], R=[CWT, b2s], W=[py])
                        p.cp("act", ac[:, hf * 512:(hf + 1) * 512], py[:], R=[py], W=[ac])
                    for k in range(4):
                        p.stt("dve", ac[:], gs[k][:], CW4[:, t, k:k + 1], ac[:], ALU.mult, ALU.add, R=[gs[k], CW4, ac], W=[ac])
                    p.tt("dve", ac[:], ac[:], GTF[:], ALU.mult, R=[ac, GTF], W=[ac])
                    p.tt("pool", x2[:], x2[:], ac[:], ALU.add, R=[x2, ac], W=[x2])
                    p.memset("pool", ssf[:], 0.0, W=[ssf])
                    p.act(junkf[:], x2[:], AF.Square, R=[x2], W=[junkf, ssf], accum_out=ssf[:])
                    p.ts("dve", rsf[:], ssf[:], 1.0 / D, EPS, ALU.mult, ALU.add, R=[ssf], W=[rsf])
                    p.act(rsf[:], rsf[:], AF.Sqrt, R=[rsf], W=[rsf])
                    p.op("dve", lambda e, rsf=rsf: e.reciprocal(rsf[:], rsf[:]), R=[rsf], W=[rsf])
                    p.stt("dve", ac[:], x2[:], rsf[:], GFIN[:], ALU.mult, ALU.mult, R=[x2, rsf, GFIN], W=[ac])
                    p.dma("sp", y[t * 128:(t + 1) * 128, :], ac[:], "st", R=[ac], W=[dY])
                p.flush()
    return nc


def _t5_buckets(dist):
    n = np.maximum(dist, 0)
    nf = np.maximum(n, 1).astype(np.float32)
    large = 16 + (np.log(nf / np.float32(16)) / np.float32(np.log(8.0)) * np.float32(16)).astype(np.int32)
    large = np.minimum(large, 31)
    return np.where(n < 16, n, large)


_NC_CACHE = {}


def kernel(x, c, w_ada, b_ada, g_mix, w_in, b_forget, sinks, rel_bias, w_proj_a, w_proj_b, w_out,
           g_ffn, w_router, b_router, w_e1, b_e1, w_e2, b_e2, g_final):
    f32 = np.float32
    A = lambda a: np.ascontiguousarray(np.asarray(a, dtype=f32))
    x2 = A(x)[0]
    xb = x2.reshape(128, 128, D)
    k = np.arange(128)[:, None]
    q = np.arange(128)[None, :]
    dist_halo = 128 + q - k
    dist_own = q - k
    rb = A(rel_bias)
    swa_bias = np.zeros((128, 2, 8, 128), f32)
    swa_bias[:, 0] = rb[_t5_buckets(dist_halo)].transpose(0, 2, 1)
    swa_bias[:, 1] = rb[_t5_buckets(dist_own)].transpose(0, 2, 1)
    swa_mask = np.zeros((128, 2, 128), f32)
    swa_mask[:, 0] = np.where((dist_halo >= 0) & (dist_halo < 128), 0.0, NEG)
    swa_mask[:, 1] = np.where((dist_own >= 0) & (dist_own < 128), 0.0, NEG)
    w1 = A(w_e1)[0]
    b1 = A(b_e1)[0]
    shared = {
        "x_all": x2,
        "c_t": A(np.asarray(c, f32).reshape(8, 128).T),
        "w_ada": A(w_ada)[0], "b_ada": A(b_ada).reshape(1, -1), "g_mix": A(g_mix).reshape(1, -1),
        "w_in": A(w_in)[0], "b_forget": A(b_forget).reshape(1, 8), "sinks": A(sinks).reshape(1, 8),
        "swa_bias": swa_bias.reshape(128, -1), "swa_mask": swa_mask.reshape(128, -1),
        "w_pa": A(w_proj_a)[0], "w_pb": A(w_proj_b)[0], "w_out": A(w_out)[0],
        "g_ffn": A(g_ffn).reshape(1, -1), "w_router": A(w_router)[0], "b_router": A(b_router).reshape(1, 32),
        "w1g": A(w1[:, :, 0::2]), "w1l": A(w1[:, :, 1::2]),
        "b1g_rows": A(b1[:, 0::2].reshape(32, 8, 128).transpose(0, 2, 1).reshape(4096, 8)),
        "b1l_rows": A(b1[:, 1::2].reshape(32, 8, 128).transpose(0, 2, 1).reshape(4096, 8)),
        "mconst": A(np.concatenate([np.tile((np.arange(48) * 512.0)[None, :], (128, 1)),
                                    (np.arange(8)[None, :] * 128.0 + np.arange(128)[:, None]),
                                    np.arange(128, dtype=f32)[:, None]], axis=1)),
        "tris": np.triu(np.ones((128, 128), f32), 1),
        "w2": A(w_e2)[0], "b2": A(b_e2)[0], "g_final": A(g_final).reshape(1, -1),
        "ident": np.eye(128, dtype=f32),
        "tri": np.triu(np.ones((128, 128), f32)),
    }
    in_maps = []
    for i in range(NCORES):
        own = xb[i::8]
        halo_ids = np.arange(16) * 8 + i - 1
        halo = np.zeros((16, 128, D), f32)
        for m_, j in enumerate(halo_ids):
            if j >= 0:
                halo[m_] = xb[j]
        kp = np.arange(128)[:, None, None]
        kb8 = np.arange(8)[None, :, None]
        qq = np.arange(128)[None, None, :]
        fm = np.where(kb8 * 128 + kp <= i * 128 + qq, BIG, NEG).astype(f32)
        meta = np.zeros((128, 24), f32)
        meta[:, i] = 1.0
        if i == 0:
            meta[:, 8] = NEG
        d = dict(shared)
        d["x_own"] = A(own.reshape(2048, D))
        d["x_halo"] = A(halo.reshape(2048, D))
        d["fmask"] = fm.reshape(128, -1)
        d["meta"] = meta
        in_maps.append(d)
    if "nc" not in _NC_CACHE:
        _NC_CACHE["nc"] = build(STAGE)
    nc = _NC_CACHE["nc"]
    res = run_bass_kernel_spmd(nc, in_maps, core_ids=list(range(NCORES)))
    if DEBUG:
        _NC_CACHE["res"] = res
    out = np.zeros((128, 128, D), f32)
    for i in range(NCORES):
        out[i::8] = res.results[i]["y"].reshape(16, 128, D)
    return out.reshape(1, S, D)
```

```python
import numpy as np
from contextlib import ExitStack
import concourse.bass as bass
import concourse.mybir as mybir
from concourse.bass_utils import run_bass_kernel_spmd

F32 = mybir.dt.float32
BF16 = mybir.dt.bfloat16
I32 = mybir.dt.int32
AF = mybir.ActivationFunctionType
ALU = mybir.AluOpType
AX = mybir.AxisListType


class Buf:
    __slots__ = ("name", "w", "r")

    def __init__(self, name=""):
        self.name = name
        self.w = None
        self.r = []


class Tn:
    def __init__(self, t, name=""):
        self.t = t
        self.b = Buf(name)

    def __getitem__(self, k):
        return self.t[k]


class Prog:
    ENG = ("pe", "act", "dve", "pool", "sp")

    def __init__(self, nc, es):
        self.nc = nc
        self.es = es
        self.sems = {e: es.enter_context(nc.semaphore("s_" + e)) for e in self.ENG}
        self.cnt = {e: 0 for e in self.ENG}
        self.chans = {}
        self.touched = set()
        self.streams = {e: [] for e in self.ENG}
        self.nphase = 0

    def chan(self, name):
        if name not in self.chans:
            self.chans[name] = [self.es.enter_context(self.nc.semaphore("c_" + name)), 0]
        return self.chans[name]

    def op(self, eng, fn, R=(), W=(), dma=None):
        deps = set()
        for b in R:
            b = b.b if isinstance(b, Tn) else b
            if b.w is not None:
                deps.add(b.w)
        for b in W:
            b = b.b if isinstance(b, Tn) else b
            if b.w is not None:
                deps.add(b.w)
            for t in b.r:
                deps.add(t)
        st = self.streams[eng]
        if dma is None:
            tok = ("e", eng, len(st))
            if eng == "pe":
                deps = {d for d in deps if not (d[0] == "e" and d[1] == "pe")}
        else:
            if eng == "pool":
                self.pool_rr = getattr(self, "pool_rr", 0) + 1
                dma = "pq%d" % (self.pool_rr % 24)
                ch = self.chan(dma)
                if ch[1] > 0:
                    deps.add(("d", dma, ch[1] - 1))
            ch = self.chan(dma)
            tok = ("d", dma, ch[1])
            ch[1] += 1
        st.append({"fn": fn, "deps": deps, "sig": False, "dma": dma})
        for b in R:
            b = b.b if isinstance(b, Tn) else b
            b.r.append(tok)
            self.touched.add(b)
        for b in W:
            b = b.b if isinstance(b, Tn) else b
            b.w = tok
            b.r = []
            self.touched.add(b)
        return tok

    def flush(self):
        nc = self.nc
        streams = self.streams
        for e in self.ENG:
            for ins in streams[e]:
                for d in ins["deps"]:
                    if d[0] == "e":
                        streams[d[1]][d[2]]["sig"] = True
        for e in self.ENG:
            for ins in reversed(streams[e]):
                if ins["dma"] is None:
                    ins["sig"] = True
                    break
        val = {}
        for e in self.ENG:
            c = self.cnt[e]
            for i, ins in enumerate(streams[e]):
                if ins["dma"] is None and ins["sig"]:
                    c += 1
                val[(e, i)] = c
            self.cnt[e] = c
        final_cnt = dict(self.cnt)
        final_ch = {k: 16 * v[1] for k, v in self.chans.items()}
        sems = self.sems
        chans = self.chans

        def emit(e, eng):
            waited = {}
            for ins in streams[e]:
                for d in sorted(ins["deps"]):
                    if d[0] == "e":
                        key = ("e", d[1])
                        v = val[(d[1], d[2])]
                        sem = sems[d[1]]
                    else:
                        key = ("d", d[1])
                        v = 16 * (d[2] + 1)
                        sem = chans[d[1]][0]
                    if waited.get(key, -1) >= v:
                        continue
                    eng.wait_ge(sem, v)
                    waited[key] = v
                bi = ins["fn"](eng)
                if ins["dma"] is not None:
                    bi.then_inc(chans[ins["dma"]][0], 16)
                elif ins["sig"]:
                    bi.then_inc(sems[e], 1)
            for o in self.ENG:
                if o != e and final_cnt[o] > 0:
                    eng.wait_ge(sems[o], final_cnt[o])
            if final_cnt[e] > 0:
                eng.wait_ge(sems[e], final_cnt[e])
            for k, v in final_ch.items():
                if v > 0:
                    eng.wait_ge(chans[k][0], v)

        with nc.Block() as blk:
            @blk.tensor
            def _(eng):
                emit("pe", eng)

            @blk.scalar
            def _(eng):
                emit("act", eng)

            @blk.vector
            def _(eng):
                emit("dve", eng)

            @blk.gpsimd
            def _(eng):
                emit("pool", eng)

            @blk.sync
            def _(eng):
                emit("sp", eng)

        self.streams = {e: [] for e in self.ENG}
        for b in self.touched:
            b.w = None
            b.r = []
        self.touched = set()
        self.nphase += 1

    def mm(self, out, lhsT, rhs, start=True, stop=True, R=(), W=()):
        return self.op("pe", lambda e: e.matmul(out, lhsT, rhs, start=start, stop=stop), R, W)

    def tr(self, out, in_, ident, R=(), W=()):
        return self.op("pe", lambda e: e.transpose(out, in_, ident), R, W)

    def act(self, out, in_, func, R=(), W=(), **kw):
        return self.op("act", lambda e: e.activation(out, in_, func, **kw), R, W)

    def ts(self, eng, out, in0, s1, s2, op0, op1=None, R=(), W=(), **kw):
        if op1 is None:
            return self.op(eng, lambda e: e.tensor_single_scalar(out, in0, s1, op0), R, W)
        return self.op(eng, lambda e: e.tensor_scalar(out, in0, s1, s2, op0, op1, **kw), R, W)

    def tt(self, eng, out, in0, in1, op, R=(), W=()):
        return self.op(eng, lambda e: e.tensor_tensor(out, in0, in1, op), R, W)

    def stt(self, eng, out, in0, scalar, in1, op0, op1, R=(), W=()):
        return self.op(eng, lambda e: e.scalar_tensor_tensor(out, in0, scalar, in1, op0, op1), R, W)

    def cp(self, eng, out, in_, R=(), W=()):
        if eng == "act":
            return self.op("act", lambda e: e.activation(out, in_, AF.Copy), R, W)
        return self.op(eng, lambda e: e.tensor_copy(out, in_), R, W)

    def memset(self, eng, ap, v, W=()):
        return self.op(eng, lambda e: e.memset(ap, v), (), W)

    def dma(self, q, out, in_, chan, R=(), W=()):
        return self.op(q, lambda e: e.dma_start(out=out, in_=in_), R, W, dma=chan)


S = 16384
D = 1024
NBLK = 128
OWN = 16
NCORES = 8
EPS = 1e-5
NEG = -30000.0
BIG = 3.0e38
STAGE = 99
DEBUG = False


def build(stage=99, dense_experts=32):
    nc = bass.Bass("TRN2", target_bir_lowering=False)

    def din(name, shape, dt=F32):
        return nc.dram_tensor(name, list(shape), dt, kind="ExternalInput").ap()

    def dscr(name, shape, dt):
        kind = "ExternalOutput" if (DEBUG and name in ("X1",)) else "Internal"
        return nc.dram_tensor(name, list(shape), dt, kind=kind).ap()

    x_all = din("x_all", [S, D])
    x_own = din("x_own", [2048, D])
    x_halo = din("x_halo", [2048, D])
    c_t = din("c_t", [128, 8])
    w_ada = din("w_ada", [D, 6 * D])
    b_ada = din("b_ada", [1, 6 * D])
    g_mix = din("g_mix", [1, D])
    w_in = din("w_in", [D, 4360])
    b_forget = din("b_forget", [1, 8])
    sinks = din("sinks", [1, 8])
    swa_bias = din("swa_bias", [128, 2 * 8 * 128])
    swa_mask = din("swa_mask", [128, 2 * 128])
    w_pa = din("w_pa", [512, D])
    w_pb = din("w_pb", [512, D])
    w_out = din("w_out", [D, D])
    g_ffn = din("g_ffn", [1, D])
    w_router = din("w_router", [D, 32])
    b_router = din("b_router", [1, 32])
    w1g = din("w1g", [32, D, D])
    w1l = din("w1l", [32, D, D])
    w2 = din("w2", [32, D, D])
    b2 = din("b2", [32, D])
    g_final = din("g_final", [1, D])
    ident = din("ident", [128, 128])
    tri = din("tri", [128, 128])
    fmask = din("fmask", [128, 8 * 128])
    meta = din("meta", [128, 24])
    mconst = din("mconst", [128, 48 + 9])
    tris = din("tris", [128, 128])
    b1g_rows = din("b1g_rows", [4096, 8])
    b1l_rows = din("b1l_rows", [4096, 8])
    y = nc.dram_tensor("y", [2048, D], F32, kind="ExternalOutput").ap()

    Ks = dscr("Ks", [8, 64, S], BF16)
    Kc = dscr("Kc", [24, S], BF16)
    Vs = dscr("Vs", [8, 128, 128, 65], BF16)
    Qs = dscr("Qs", [8, 64, 2048], BF16)
    Qc = dscr("Qc", [24, 2048], BF16)
    YA = dscr("YA", [512, 2048], BF16)
    YB = dscr("YB", [8, 64, 2048], BF16)
    X1 = dscr("X1", [2048, D], F32)
    Xs = dscr("Xs", [48 * 512, D], BF16)
    Ys = dscr("Ys", [48 * 512, D], F32)
    dXs, dYs = Buf("Xs"), Buf("Ys")
    DBG = nc.dram_tensor("DBG", [128, 256], F32, kind="ExternalOutput").ap() if DEBUG else None
    dKs, dKc, dVs, dQs, dQc, dYA, dYB, dX1, dY = (Buf(n) for n in "Ks Kc Vs Qs Qc YA YB X1 Y".split())

    with ExitStack() as es:
        p = Prog(nc, es)

        def sb(name, shape, dt, st=es):
            return Tn(st.enter_context(nc.sbuf_tensor(name, shape, dt)), name)

        def ps(name, shape, dt, st=es):
            return Tn(st.enter_context(nc.psum_tensor(name, shape, dt)), name)

        PSA = [ps("psa%d" % i, [128, 512], F32) for i in range(6)]
        PST = [ps("pst%d" % i, [128, 1024], BF16) for i in range(2)]
        rot = {"psa": 0, "pst": 0, "n": 0}

        def psa():
            rot["psa"] += 1
            return PSA[rot["psa"] % 6]

        def pst():
            rot["pst"] += 1
            return PST[rot["pst"] % 2]

        idb = sb("idb", [128, 128], BF16)
        idf = sb("idf", [128, 128], F32)
        SHF = sb("SHF", [128, D], F32)
        GF = sb("GF", [128, D], F32)
        GTF = sb("GTF", [128, D], F32)
        GFIN = sb("GFIN", [128, D], F32)
        esink = sb("esink", [128, 8], F32)
        bfb = sb("bfb", [128, 4, 8], F32)
        brb = sb("brb", [128, 32], F32)
        epsc = sb("epsc", [128, 1], F32)
        onec = sb("onec", [128, 1], F32)
        metat = sb("metat", [128, 24], F32)
        onesf = sb("onesf", [128, 128], F32)
        esm = ExitStack()
        SHM = sb("SHM", [128, D], F32, esm)
        GM = sb("GM", [128, D], F32, esm)
        GTM = sb("GTM", [128, D], F32, esm)

        with ExitStack() as ph:
            cT = sb("cT", [128, 8], F32, ph)
            cact = sb("cact", [128, 8], F32, ph)
            onesb = sb("onesb", [128, 128], BF16, ph)
            cbs = sb("cbs", [128, 8, 128], BF16, ph)
            wada = [sb("wada%d" % i, [128, 8, 1024], BF16, ph) for i in range(2)]
            bada = sb("bada", [128, 6 * D], F32, ph)
            gmb = sb("gmb", [128, D], F32, ph)
            gfb = sb("gfb", [128, D], F32, ph)
            snk = sb("snk", [128, 8], F32, ph)
            p.dma("pool", idb[:], ident, "cast", W=[idb])
            p.dma("sp", idf[:], ident, "ld", W=[idf])
            p.dma("sp", cT[:], c_t, "ld", W=[cT])
            p.dma("sp", bada[:], b_ada.partition_broadcast(128), "ld", W=[bada])
            p.dma("sp", gmb[:], g_mix.partition_broadcast(128), "ld", W=[gmb])
            p.dma("sp", gfb[:], g_ffn.partition_broadcast(128), "ld", W=[gfb])
            p.dma("sp", GFIN[:], g_final.partition_broadcast(128), "ld", W=[GFIN])
            p.dma("sp", snk[:], sinks.partition_broadcast(128), "ld", W=[snk])
            for t4 in range(4):
                p.dma("sp", bfb[:, t4, :], b_forget.partition_broadcast(128), "ld", W=[bfb])
            p.dma("sp", brb[:], b_router.partition_broadcast(128), "ld", W=[brb])
            p.dma("sp", metat[:], meta, "ld", W=[metat])
            p.memset("dve", epsc[:], EPS, W=[epsc])
            p.memset("dve", onec[:], 1.0, W=[onec])
            p.memset("dve", onesf[:], 1.0, W=[onesf])
            p.memset("dve", onesb[:], 1.0, W=[onesb])
            p.act(esink[:], snk[:], AF.Exp, R=[snk], W=[esink])
            p.act(cact[:], cT[:], AF.Silu, R=[cT], W=[cact])
            for kc in range(8):
                p.ts("dve", cbs[:, kc, :], onesb[:], cact[:, kc:kc + 1], None, ALU.mult, R=[onesb, cact], W=[cbs])
            dst = [SHM, GM, GTM, SHF, GF, GTF]
            for n in range(6):
                wa = wada[n % 2]
                p.dma("pool", wa[:], w_ada[:, n * D:(n + 1) * D].rearrange("(k p) n -> p k n", p=128), "cast", W=[wa])
                for hf in range(2):
                    pa = psa()
                    for kc in range(8):
                        p.mm(pa[:], cbs[:, kc, :], wa[:, kc, hf * 512:(hf + 1) * 512], start=(kc == 0), stop=(kc == 7),
                             R=[cbs, wa], W=[pa])
                    p.tt("dve", dst[n][:, hf * 512:(hf + 1) * 512], pa[:], bada[:, n * D + hf * 512:n * D + (hf + 1) * 512],
                         ALU.add, R=[pa, bada], W=[dst[n]])
            p.stt("dve", GM[:], GM[:], 1.0, gmb[:], ALU.add, ALU.mult, R=[GM, gmb], W=[GM])
            p.stt("dve", GF[:], GF[:], 1.0, gfb[:], ALU.add, ALU.mult, R=[GF, gfb], W=[GF])
            p.flush()

        def mk_norm(ph, tag, nx=3, nt=2, nu=2):
            XT = [sb("xt%s%d" % (tag, i), [128, D], F32, ph) for i in range(nx)]
            TMP = [sb("tmp%s%d" % (tag, i), [128, D], F32, ph) for i in range(nt)]
            U = [sb("u%s%d" % (tag, i), [128, D], BF16, ph) for i in range(nu)]
            junk = sb("junk" + tag, [128, D], BF16, ph)
            SSq = [sb("ss%s%d" % (tag, i), [128, 1], F32, ph) for i in range(3)]
            RS = [sb("rs%s%d" % (tag, i), [128, 1], F32, ph) for i in range(3)]

            def norm_a1(src, srcbuf, G, SH):
                i = rot["n"]
                rot["n"] += 1
                xt, tmp, u, ss, rs = XT[i % nx], TMP[i % nt], U[i % nu], SSq[i % 3], RS[i % 3]
                p.dma("sp", xt[:], src, "ldx", R=srcbuf, W=[xt])
                p.memset("pool", ss[:], 0.0, W=[ss])
                p.act(junk[:], xt[:], AF.Square, R=[xt], W=[junk, ss], accum_out=ss[:])
                p.ts("dve", rs[:], ss[:], 1.0 / D, EPS, ALU.mult, ALU.add, R=[ss], W=[rs])
                p.act(rs[:], rs[:], AF.Sqrt, R=[rs], W=[rs])
                p.op("dve", lambda e: e.reciprocal(rs[:], rs[:]), R=[rs], W=[rs])
                p.stt("dve", tmp[:], xt[:], rs[:], G[:], ALU.mult, ALU.mult, R=[xt, rs, G], W=[tmp])
                p.tt("pool", u[:], tmp[:], SH[:], ALU.add, R=[tmp, SH], W=[u])
                return (xt, u)

            def norm_a2(hd, uT, col0, uTbuf=None):
                xt, u = hd
                pt = pst()
                for kc in range(8):
                    p.tr(pt[:, kc * 128:(kc + 1) * 128], u[:, kc * 128:(kc + 1) * 128], idb[:], R=[u, idb], W=[pt])
                p.cp("act", uT[:, :, col0:col0 + 128], pt[:].rearrange("p (k t) -> p k t", k=8), R=[pt],
                     W=[uT if uTbuf is None else uTbuf])
                return xt

            def norm_tile(src, srcbuf, G, SH, uT, col0, uTbuf=None):
                return norm_a2(norm_a1(src, srcbuf, G, SH), uT, col0, uTbuf)
            norm_tile.a1 = norm_a1
            norm_tile.a2 = norm_a2
            return norm_tile

        with ExitStack() as ph:
            norm_tile = mk_norm(ph, "a", 3, 2, 3)
            wkv = sb("wkv", [128, 8, 1032], BF16, ph)
            UT = [sb("uTa%d" % i, [128, 8, 512], BF16, ph) for i in range(2)]
            VSB = [sb("vsb%d" % i, [128, 8, 4, 65], BF16, ph) for i in range(2)]
            KSB = [sb("ksb%d" % i, [128, 512], BF16, ph) for i in range(2)]
            LF = sb("LF", [128, 128, 8], F32, ph)
            fa = sb("fa", [128, 32], F32, ph)
            fb_ = sb("fb_", [128, 32], F32, ph)
            fc = sb("fc", [128, 32], F32, ph)
            fd = sb("fd", [128, 32], F32, ph)
            psF = PSA[5]
            p.dma("pool", wkv[:], w_in[:, 1280:2312].rearrange("(k p) n -> p k n", p=128), "cast", W=[wkv])
            for v in VSB:
                p.memset("pool", v[:], 1.0, W=[v])
            ngroups = 32 if stage >= 1 else 1
            ntile = ngroups * 4
            UTB = [[Buf('utb%d_%d' % (a_, b_)) for b_ in range(4)] for a_ in range(2)]

            hds = {}

            def partA1(T):
                hds[T] = norm_tile.a1(x_all[T * 128:(T + 1) * 128, :], [], GM, SHM)

            def partA2(T):
                g, t = divmod(T, 4)
                norm_tile.a2(hds.pop(T), UT[g % 2], t * 128, UTB[g % 2][t])

            def partB(T):
                g, t = divmod(T, 4)
                uT = UT[g % 2]
                ub = UTB[g % 2][t]
                uball = UTB[g % 2]
                vsb = VSB[g % 2]
                rot["psa"] += 1
                pv = PSA[rot["psa"] % 5]
                for kc in range(8):
                    p.mm(pv[:], uT[:, kc, t * 128:(t + 1) * 128], wkv[:, kc, 512:1024], start=(kc == 0), stop=(kc == 7),
                         R=[ub, wkv], W=[pv])
                p.cp("act", vsb[:, :, t, 0:64], pv[:].rearrange("p (h d) -> p h d", h=8), R=[pv], W=[vsb])
                for kc in range(8):
                    p.mm(psF[:, t * 8:(t + 1) * 8], uT[:, kc, t * 128:(t + 1) * 128], wkv[:, kc, 1024:1032],
                         start=(kc == 0), stop=(kc == 7), R=[ub, wkv], W=[psF])
                if t < 3:
                    return
                for cj in range(4):
                    rot["psa"] += 1
                    pk = PSA[rot["psa"] % 5]
                    for kc in range(8):
                        p.mm(pk[:], wkv[:, kc, cj * 128:(cj + 1) * 128], uT[:, kc, :], start=(kc == 0), stop=(kc == 7),
                             R=uball + [wkv], W=[pk])
                    ksb = KSB[cj % 2]
                    p.cp("act", ksb[:], pk[:], R=[pk], W=[ksb])
                    for hh in range(2):
                        p.dma("act", Ks[2 * cj + hh, :, g * 512:(g + 1) * 512], ksb[hh * 64:(hh + 1) * 64, :], "st",
                              R=[ksb], W=[dKs])
                p.dma("act", Vs[:, :, 4 * g:4 * g + 4, :].rearrange("h p b c -> p h b c"), vsb[:], "st", R=[vsb], W=[dVs])
                p.tt("dve", fa[:], psF[:, 0:32], bfb[:].rearrange("p a b -> p (a b)"), ALU.add, R=[psF, bfb], W=[fa])
                p.stt("dve", fb_[:], fa[:], -1.0, fa[:], ALU.mult, ALU.max, R=[fa], W=[fb_])
                p.act(fc[:], fb_[:], AF.Exp, R=[fb_], W=[fc], scale=-1.0)
                p.act(fc[:], fc[:], AF.Ln, R=[fc, onec], W=[fc], bias=onec[:])
                p.ts("dve", fd[:], fa[:], 0.0, None, ALU.min, R=[fa], W=[fd])
                p.tt("dve", LF[:, 4 * g:4 * g + 4, :], fd[:].rearrange("p (a b) -> p a b", a=4),
                     fc[:].rearrange("p (a b) -> p a b", a=4), ALU.subtract, R=[fd, fc], W=[LF])

            partA1(0)
            if ntile > 1:
                partA1(1)
            partA2(0)
            for T in range(ntile):
                if T + 2 < ntile:
                    partA1(T + 2)
                if T + 1 < ntile:
                    partA2(T + 1)
                partB(T)
            CI = sb("CI", [128, 1024], F32, ph)
            TOTa = sb("TOTa", [128, 128, 8], F32, ph)
            TOTb = sb("TOTb", [128, 128, 8], F32, ph)
            TOT0 = sb("TOT0", [128, 128, 8], F32, ph)
            CUM = sb("CUM", [128, 128, 8], F32, ph)
            R1 = sb("R1", [128, 1024], F32, ph)
            NN = sb("NN", [128, 128, 24], BF16, ph)
            ckT = sb("ckT", [24, S], BF16, ph)
            trif = sb("trif", [128, 128], F32, ph)
            p.dma("sp", trif[:], tri, "ld", W=[trif])
            LFf = LF[:].rearrange("p a b -> p (a b)")
            for hf in range(2):
                pa = psa()
                p.mm(pa[:], trif[:], LFf[:, hf * 512:(hf + 1) * 512], R=[trif, LF], W=[pa])
                p.cp("act", CI[:, hf * 512:(hf + 1) * 512], pa[:], R=[pa], W=[CI])
                pb = psa()
                p.mm(pb[:], onesf[:], LFf[:, hf * 512:(hf + 1) * 512], R=[onesf, LF], W=[pb])
                p.cp("act", TOT0[:].rearrange("p a b -> p (a b)")[:, hf * 512:(hf + 1) * 512], pb[:], R=[pb], W=[TOT0])
            p.cp("dve", TOTa[:], TOT0[:], R=[TOT0], W=[TOTa])
            a, b = TOTa, TOTb
            s = 1
            while s < 128:
                p.tt("dve", b[:, s:, :], a[:, s:, :], a[:, :128 - s, :], ALU.add, R=[a], W=[b])
                p.cp("dve", b[:, :s, :], a[:, :s, :], R=[a], W=[b])
                a, b = b, a
                s *= 2
            p.tt("dve", b[:], a[:], TOT0[:], ALU.subtract, R=[a, TOT0], W=[b])
            p.tt("dve", CUM[:], CI[:].rearrange("p (a b) -> p a b", b=8), b[:], ALU.add, R=[CI, b], W=[CUM])

            def split3(src3, neg, dstNN, nb):
                sgn = -1.0 if neg else 1.0
                r1 = R1[:, 0:nb * 8].rearrange("p (a b) -> p a b", b=8)
                p.ts("dve", dstNN[:, :, 0:8], src3, sgn, None, ALU.mult, R=[CUM, OCt], W=[dstNN])
                p.stt("dve", r1, src3, sgn, dstNN[:, :, 0:8], ALU.mult, ALU.subtract, R=[CUM, OCt, dstNN], W=[R1])
                p.cp("dve", dstNN[:, :, 8:16], r1, R=[R1], W=[dstNN])
                p.tt("dve", r1, r1, dstNN[:, :, 8:16], ALU.subtract, R=[R1, dstNN], W=[R1])
                p.cp("dve", dstNN[:, :, 16:24], r1, R=[R1], W=[dstNN])

            OCt = sb("OCt", [128, 16, 8], F32, ph)
            QN = sb("QN", [128, 16, 24], BF16, ph)
            cqT = sb("cqT", [24, 2048], BF16, ph)
            split3(CUM[:], True, NN, 128)
            for b8 in range(16):
                pt = pst()
                for j in range(8):
                    blk = b8 * 8 + j
                    p.tr(pt[0:24, j * 128:(j + 1) * 128], NN[:, blk, :], idb[:], R=[NN, idb], W=[pt])
                p.cp("act", ckT[:, b8 * 1024:(b8 + 1) * 1024], pt[0:24, :], R=[pt], W=[ckT])
            p.dma("sp", Kc, ckT[:], "st", R=[ckT], W=[dKc])
            CUM4 = CUM[:].rearrange("p (m r) h -> p m r h", r=8)
            p.ts("dve", OCt[:], CUM4[:, :, 0, :], metat[:, 0:1], None, ALU.mult, R=[CUM, metat], W=[OCt])
            for r in range(1, 8):
                p.stt("dve", OCt[:], CUM4[:, :, r, :], metat[:, r:r + 1], OCt[:], ALU.mult, ALU.add, R=[CUM, metat, OCt], W=[OCt])
            split3(OCt[:], False, QN, 16)
            for b8 in range(2):
                pt = pst()
                for j in range(8):
                    p.tr(pt[0:24, j * 128:(j + 1) * 128], QN[:, b8 * 8 + j, :], idb[:], R=[QN, idb], W=[pt])
                p.cp("act", cqT[:, b8 * 1024:(b8 + 1) * 1024], pt[0:24, :], R=[pt], W=[cqT])
            p.dma("sp", Qc, cqT[:], "st", R=[cqT], W=[dQc])
            p.flush()

        with ExitStack() as ph:
            norm_tile = mk_norm(ph, "c")
            wq = sb("wq", [128, 8, 1280], BF16, ph)
            UTo = sb("uTo", [128, 8, 512], BF16, ph)
            UTh = sb("uTh", [128, 8, 512], BF16, ph)
            QSB = [sb("qsb%d" % i, [128, 512], BF16, ph) for i in range(2)]
            QA = sb("QA", [64, 8, 512], BF16, ph)
            KA = sb("KA", [64, 2, 2, 512], BF16, ph)
            VA = sb("VA", [128, 2, 4, 2, 65], BF16, ph)
            BM = sb("BM", [128, 2, 8, 128], F32, ph)
            MK = sb("MK", [128, 2, 128], F32, ph)
            SBI = [sb("sbi%d" % i, [128, 512], F32, ph) for i in range(2)]
            PTs = [sb("pts%d" % i, [128, 512], BF16, ph) for i in range(2)]
            den = sb("den", [128, 8], F32, ph)
            yat = sb("yat", [128, 512], BF16, ph)
            YAT = sb("YATt", [128, 4, 128], BF16, ph)
            p.dma("pool", wq[:], w_in[:, 0:1280].rearrange("(k p) n -> p k n", p=128), "cast", W=[wq])
            p.dma("sp", BM[:].rearrange("p a h q -> p (a h q)"), swa_bias, "ld", W=[BM])
            p.dma("sp", MK[:].rearrange("p a q -> p (a q)"), swa_mask, "ld", W=[MK])
            for hf in range(2):
                for h in range(8):
                    p.tt("dve", BM[:, hf, h, :], BM[:, hf, h, :], MK[:, hf, :], ALU.add, R=[BM, MK], W=[BM])
            p.memset("pool", VA[:], 1.0, W=[VA])
            for gq in range(4 if stage >= 2 else 0):
                for t in range(4):
                    m = 4 * gq + t
                    norm_tile(x_own[m * 128:(m + 1) * 128, :], [], GM, SHM, UTo, t * 128)
                    norm_tile(x_halo[m * 128:(m + 1) * 128, :], [], GM, SHM, UTh, t * 128)
                for cj in range(4):
                    pq = psa()
                    for kc in range(8):
                        p.mm(pq[:], wq[:, kc, 768 + cj * 128:768 + (cj + 1) * 128], UTo[:, kc, :], start=(kc == 0), stop=(kc == 7),
                             R=[wq, UTo], W=[pq])
                    qsb = QSB[cj % 2]
                    p.act(qsb[:], pq[:], AF.Copy, R=[pq], W=[qsb], scale=0.125)
                    for hh in range(2):
                        p.dma("sp", Qs[2 * cj + hh, :, gq * 512:(gq + 1) * 512], qsb[hh * 64:(hh + 1) * 64, :], "st",
                              R=[qsb], W=[dQs])
                for h in range(8):
                    pq = psa()
                    for kc in range(8):
                        p.mm(pq[0:64, :], wq[:, kc, h * 64:(h + 1) * 64], UTo[:, kc, :], start=(kc == 0), stop=(kc == 7),
                             R=[wq, UTo], W=[pq])
                    p.act(QA[:, h, :], pq[0:64, :], AF.Copy, R=[pq], W=[QA], scale=0.125)
                for si, UTs in enumerate((UTh, UTo)):
                    for kvh in range(2):
                        pq = psa()
                        for kc in range(8):
                            p.mm(pq[0:64, :], wq[:, kc, 512 + kvh * 64:512 + (kvh + 1) * 64], UTs[:, kc, :],
                                 start=(kc == 0), stop=(kc == 7), R=[wq, UTs], W=[pq])
                        p.cp("act", KA[:, si, kvh, :], pq[0:64, :], R=[pq], W=[KA])
                    for t in range(4):
                        pq = psa()
                        for kc in range(8):
                            p.mm(pq[:, 0:128], UTs[:, kc, t * 128:(t + 1) * 128], wq[:, kc, 640:768],
                                 start=(kc == 0), stop=(kc == 7), R=[wq, UTs], W=[pq])
                        p.cp("dve", VA[:, si, t, :, 0:64], pq[:, 0:128].rearrange("p (a d) -> p a d", a=2), R=[pq], W=[VA])
                for t in range(4):
                    m = 4 * gq + t
                    pos = [psa(), psa()]
                    for kvh in range(2):
                        po = pos[kvh]
                        pTs = []
                        for si in range(2):
                            pS = psa()
                            p.mm(pS[:], KA[:, si, kvh, t * 128:(t + 1) * 128], QA[:, kvh * 4:(kvh + 1) * 4, t * 128:(t + 1) * 128],
                                 R=[KA, QA], W=[pS])
                            sbi = SBI[si]
                            p.tt("dve", sbi[:], pS[:], BM[:, si, kvh * 4:(kvh + 1) * 4, :].rearrange("p h q -> p (h q)"),
                                 ALU.add, R=[pS, BM], W=[sbi])
                            pT = PTs[si]
                            if si == 0:
                                p.act(pT[:], sbi[:], AF.Exp, R=[sbi, metat], W=[pT], bias=metat[:, 8 + m:9 + m])
                            else:
                                p.act(pT[:], sbi[:], AF.Exp, R=[sbi], W=[pT])
                            pTs.append(pT)
                        for g4 in range(4):
                            for si in range(2):
                                p.mm(po[:, g4 * 65:(g4 + 1) * 65], pTs[si][:, g4 * 128:(g4 + 1) * 128], VA[:, si, t, kvh, :],
                                     start=(si == 0), stop=(si == 1), R=[pTs[si], VA], W=[po])
                    for kvh in range(2):
                        po = pos[kvh]
                        po3 = po[:, 0:260].rearrange("p (g c) -> p g c", c=65)
                        p.tt("dve", den[:, kvh * 4:(kvh + 1) * 4], po3[:, :, 64], esink[:, kvh * 4:(kvh + 1) * 4], ALU.add,
                             R=[po, esink], W=[den])
                        p.op("dve", lambda e, k=kvh: e.reciprocal(den[:, k * 4:(k + 1) * 4], den[:, k * 4:(k + 1) * 4]), R=[den], W=[den])
                        for g4 in range(4):
                            h = kvh * 4 + g4
                            p.ts("dve", yat[:, h * 64:(h + 1) * 64], po[:, g4 * 65:g4 * 65 + 64], den[:, h:h + 1], None, ALU.mult,
                                 R=[po, den], W=[yat])
                    pt = pst()
                    for c4 in range(4):
                        p.tr(pt[:, c4 * 128:(c4 + 1) * 128], yat[:, c4 * 128:(c4 + 1) * 128], idb[:], R=[yat, idb], W=[pt])
                    p.cp("act", YAT[:], pt[:, 0:512].rearrange("p (c t) -> p c t", c=4), R=[pt], W=[YAT])
                    p.dma("sp", YA[:, m * 128:(m + 1) * 128].rearrange("(c p) t -> p c t", p=128), YAT[:], "st", R=[YAT], W=[dYA])
            p.flush()

        with ExitStack() as ph:
            KAUG = [sb("kaug%d" % i, [70, S], BF16, ph) for i in range(2)]
            VH = [sb("vh%d" % i, [128, 128, 65], BF16, ph) for i in range(2)]
            QH = [sb("qh%d" % i, [70, 2048], BF16, ph) for i in range(2)]
            PT = [sb("pt%d" % i, [128, 512], BF16, ph) for i in range(3)]
            MSK = [sb("msk%d" % i, [128, 512], F32, ph) for i in range(2)]
            FM = sb("FM", [128, 8, 512], F32, ph)
            LR = sb("LR", [65, 512], F32, ph)
            BCS = sb("BCS", [64, 512], F32, ph)
            YBS = [sb("ybs%d" % i, [64, 512], BF16, ph) for i in range(2)]
            p.memset("pool", FM[:], BIG, W=[FM])
            p.dma("sp", FM[:, :, 0:128], fmask.rearrange("p (a q) -> p a q", a=8), "ld", W=[FM])
            for i in range(2):
                p.memset("pool", KAUG[i][64:70, :], 1.0, W=[KAUG[i]])
                p.memset("pool", QH[i][64:70, :], 1.0, W=[QH[i]])
            Kc3 = Kc.rearrange("(x h) t -> x h t", x=3)
            Qc3 = Qc.rearrange("(x h) t -> x h t", x=3)
            PSS = PSA[0:3]
            PSO = PSA[3:5]
            psBC = PSA[5]
            its = []
            gi = 0
            for h in range(8 if stage >= 3 else 0):
                for g in range(4):
                    q0 = g * 512
                    lst = []
                    for kb in range(32 * g):
                        lst.append(dict(h=h, kb=kb, n=512, c0=0, q0=q0, masked=False, kb8=0, gi=gi))
                    for r in range(4):
                        for kb8 in range(8):
                            lst.append(dict(h=h, kb=32 * g + 8 * r + kb8, n=(4 - r) * 128, c0=r * 128, q0=q0, masked=True,
                                            kb8=kb8, gi=gi))
                    lst[0]["first"] = True
                    lst[-1]["last"] = True
                    its.extend(lst)
                    gi += 1
            loaded = set()

            def load_head(h):
                if h in loaded or h >= 8:
                    return
                loaded.add(h)
                ka, vh, qh = KAUG[h % 2], VH[h % 2], QH[h % 2]
                p.dma("sp", ka[0:64, :], Ks[h], "ldk", R=[dKs], W=[ka])
                p.dma("sp", ka[67:70, :], Kc3[:, h, :], "ldk", R=[dKc], W=[ka])
                p.dma("sp", vh[:], Vs[h], "ldk", R=[dVs], W=[vh])
                p.dma("sp", qh[0:64, :], Qs[h], "ldk", R=[dQs], W=[qh])
                p.dma("sp", qh[64:67, :], Qc3[:, h, :], "ldk", R=[dQc], W=[qh])

            def emitS(i):
                d = its[i]
                h = d["h"]
                load_head(h)
                ka, qh = KAUG[h % 2], QH[h % 2]
                pS, pT = PSS[i % 3], PT[i % 3]
                n, c0, q0, kb = d["n"], d["c0"], d["q0"], d["kb"]
                p.mm(pS[:, 0:n], ka[0:70, kb * 128:(kb + 1) * 128], qh[0:70, q0 + c0:q0 + 512], R=[ka, qh], W=[pS])
                if d["masked"]:
                    mk = MSK[i % 2]
                    p.tt("dve", mk[:, 0:n], pS[:, 0:n], FM[:, d["kb8"], 0:n], ALU.min, R=[pS, FM], W=[mk])
                    p.act(pT[:, 0:n], mk[:, 0:n], AF.Exp, R=[mk], W=[pT])
                else:
                    p.act(pT[:], pS[:], AF.Exp, R=[pS], W=[pT])

            def emitPV(i):
                d = its[i]
                h = d["h"]
                vh = VH[h % 2]
                po = PSO[d["gi"] % 2]
                pT = PT[i % 3]
                n, c0, q0, kb = d["n"], d["c0"], d["q0"], d["kb"]
                p.mm(po[0:65, c0:512], vh[:, kb, :], pT[:, 0:n], start=bool(d.get("first")), stop=bool(d.get("last")),
                     R=[vh, pT], W=[po])
                if d.get("last"):
                    if q0 == 0:
                        load_head(h + 1)
                    p.cp("act", LR[64:65, :], po[64:65, :], R=[po], W=[LR])
                    p.op("dve", lambda e: e.reciprocal(LR[64:65, :], LR[64:65, :]), R=[LR], W=[LR])
                    p.mm(psBC[0:64, :], onesf[64:65, 0:64], LR[64:65, :], R=[onesf, LR], W=[psBC])
                    p.cp("act", BCS[:], psBC[0:64, :], R=[psBC], W=[BCS])
                    ybs = YBS[d["gi"] % 2]
                    p.tt("dve", ybs[:], po[0:64, :], BCS[:], ALU.mult, R=[po, BCS], W=[ybs])
                    p.dma("sp", YB[h, :, q0:q0 + 512], ybs[:], "st", R=[ybs], W=[dYB])

            LA = 2
            for i in range(len(its) + LA):
                if i < len(its):
                    emitS(i)
                if i - LA >= 0:
                    emitPV(i - LA)
            p.flush()

        with ExitStack() as ph:
            norm_tile = mk_norm(ph, "e")
            wg = sb("wg", [128, 8, 2048], BF16, ph)
            wpa = sb("wpa", [128, 4, 1024], BF16, ph)
            wpb = sb("wpb", [64, 8, 1024], BF16, ph)
            wo = sb("wo", [128, 8, 1024], BF16, ph)
            UTb = sb("uTb", [128, 8, 128], BF16, ph)
            SG = sb("SG", [128, 2048], F32, ph)
            yaTb = sb("yaTb", [128, 4, 128], BF16, ph)
            ybTb = sb("ybTb", [64, 8, 128], BF16, ph)
            T1 = sb("T1", [128, 512], F32, ph)
            T2 = sb("T2", [128, 512], F32, ph)
            MG = sb("MG", [128, 1024], BF16, ph)
            MGT = sb("MGT", [128, 8, 128], BF16, ph)
            T3 = sb("T3", [128, 1024], F32, ph)
            X1t = [sb("x1t%d" % i, [128, 1024], F32, ph) for i in range(2)]
            p.dma("pool", wg[:], w_in[:, 2312:4360].rearrange("(k p) n -> p k n", p=128), "cast", W=[wg])
            p.dma("pool", wpa[:], w_pa.rearrange("(c p) n -> p c n", p=128), "cast", W=[wpa])
            p.dma("pool", wpb[:], w_pb.rearrange("(h p) n -> p h n", p=64), "cast", W=[wpb])
            p.dma("pool", wo[:], w_out.rearrange("(k p) n -> p k n", p=128), "cast", W=[wo])
            ZT = sb("ZT", [128, 4, 1024], BF16, ph)
            p.memset("pool", ZT[:], 0.0, W=[ZT])
            for u_ in range(48):
                p.dma("sp", Xs[u_ * 512:(u_ + 1) * 512, :].rearrange("(a p) n -> p a n", p=128), ZT[:], "st", R=[ZT], W=[dXs])
            UTbs = [UTb, sb("uTb2", [128, 8, 128], BF16, ph)]
            SGs_ = [SG, sb("SG2", [128, 2048], F32, ph)]
            yaTbs = [yaTb, sb("yaTb2", [128, 4, 128], BF16, ph)]
            ybTbs = [ybTb, sb("ybTb2", [64, 8, 128], BF16, ph)]
            xts = {}

            def stage1(m):
                UTb_, SG_, ya_, yb_ = UTbs[m % 2], SGs_[m % 2], yaTbs[m % 2], ybTbs[m % 2]
                xts[m] = norm_tile(x_own[m * 128:(m + 1) * 128, :], [], GM, SHM, UTb_, 0)
                p.dma("sp", ya_[:], YA[:, m * 128:(m + 1) * 128].rearrange("(c p) t -> p c t", p=128), "ldy", R=[dYA], W=[ya_])
                p.dma("sp", yb_[:], YB[:, :, m * 128:(m + 1) * 128].rearrange("h d t -> d h t"), "ldy", R=[dYB], W=[yb_])
                for j in range(4):
                    pg = psa()
                    for kc in range(8):
                        p.mm(pg[:], UTb_[:, kc, :], wg[:, kc, j * 512:(j + 1) * 512], start=(kc == 0), stop=(kc == 7),
                             R=[UTb_, wg], W=[pg])
                    p.act(SG_[:, j * 512:(j + 1) * 512], pg[:], AF.Sigmoid, R=[pg], W=[SG_])

            def stage2(m):
                SG_, ya_, yb_ = SGs_[m % 2], yaTbs[m % 2], ybTbs[m % 2]
                xt = xts.pop(m)
                for hf in range(2):
                    pa = psa()
                    for c4 in range(4):
                        p.mm(pa[:], ya_[:, c4, :], wpa[:, c4, hf * 512:(hf + 1) * 512], start=(c4 == 0), stop=(c4 == 3),
                             R=[ya_, wpa], W=[pa])
                    pb = psa()
                    for h in range(8):
                        p.mm(pb[:], yb_[:, h, :], wpb[:, h, hf * 512:(hf + 1) * 512], start=(h == 0), stop=(h == 7),
                             R=[yb_, wpb], W=[pb])
                    p.tt("dve", T1[:], pa[:], SG_[:, hf * 512:(hf + 1) * 512], ALU.mult, R=[pa, SG_], W=[T1])
                    p.tt("dve", T2[:], pb[:], SG_[:, 1024 + hf * 512:1024 + (hf + 1) * 512], ALU.mult, R=[pb, SG_], W=[T2])
                    p.tt("pool", MG[:, hf * 512:(hf + 1) * 512], T1[:], T2[:], ALU.add, R=[T1, T2], W=[MG])
                pt = pst()
                for kc in range(8):
                    p.tr(pt[:, kc * 128:(kc + 1) * 128], MG[:, kc * 128:(kc + 1) * 128], idb[:], R=[MG, idb], W=[pt])
                p.cp("act", MGT[:], pt[:].rearrange("p (k t) -> p k t", k=8), R=[pt], W=[MGT])
                x1 = X1t[m % 2]
                for hf in range(2):
                    pw = psa()
                    for kc in range(8):
                        p.mm(pw[:], MGT[:, kc, :], wo[:, kc, hf * 512:(hf + 1) * 512], start=(kc == 0), stop=(kc == 7),
                             R=[MGT, wo], W=[pw])
                    p.tt("dve", T3[:, hf * 512:(hf + 1) * 512], pw[:], GTM[:, hf * 512:(hf + 1) * 512], ALU.mult, R=[pw, GTM], W=[T3])
                p.tt("pool", x1[:], T3[:], xt[:], ALU.add, R=[T3, xt], W=[x1])
                p.dma("sp", X1[m * 128:(m + 1) * 128, :], x1[:], "st", R=[x1], W=[dX1])

            nblk = 16 if stage >= 4 else 0
            if nblk:
                stage1(0)
            for m in range(nblk):
                if m + 1 < nblk:
                    stage1(m + 1)
                stage2(m)
            p.flush()

        esm.close()
        NU = 47
        NSLOT = NU * 512
        with ExitStack() as phF:
            CWF = sb("CWF", [128, 16, 32], F32, phF)
            CWFB = [Buf("cwf%d" % i) for i in range(16)]
            CW4 = sb("CW4", [128, 16, 4], F32, phF)
            SLI = sb("SLI", [128, 64], I32, phF)
            EU = sb("EU", [128, NU], F32, phF)
            EU1024 = sb("EU1024", [128, NU], F32, phF)
            EU128 = sb("EU128", [128, NU], F32, phF)
            mc = sb("mc", [128, 57], F32, phF)
            b2s = sb("b2s", [32, 1024], F32, phF)
            p.dma("sp", mc[:], mconst, "ld", W=[mc])
            p.dma("sp", b2s[:], b2, "ld", W=[b2s])
            uvals = mc[:, 0:NU]
            rowoff = mc[:, 48:56]
            pidx = mc[:, 56:57]
            with ExitStack() as ph:
                norm_tile = mk_norm(ph, "f", 2, 1, 1)
                U2 = [sb("U2_%d" % i, [128, 1024], BF16, ph) for i in range(16)]
                U2Tt = [sb("U2Tt%d" % i, [128, 8, 128], BF16, ph) for i in range(2)]
                wr = sb("wr", [128, 8, 32], BF16, ph)
                trs = sb("trs", [128, 128], F32, ph)
                LGs = sb("LGs", [128, 16, 32], F32, ph)
                M8s = sb("M8s", [128, 16, 8], F32, ph)
                MK = sb("MKrt", [128, 16, 32], F32, ph)
                NM = sb("NM", [128, 1], F32, ph)
                EX4 = sb("EX4", [128, 4], F32, ph)
                S4 = sb("S4", [128, 1], F32, ph)
                CNT = sb("CNT", [128, 32], F32, ph)
                PADc = sb("PADc", [128, 32], F32, ph)
                TMPc = sb("TMPc", [128, 32], F32, ph)
                ENDa = sb("ENDa", [128, 32], F32, ph)
                ENDb = sb("ENDb", [128, 32], F32, ph)
                BASE = sb("BASE", [128, 32], F32, ph)
                TU = sb("TU", [128, NU], F32, ph)
                SLT = sb("SLT", [128, 32], F32, ph)
                OH = sb("OH", [128, 32], F32, ph)
                OHS = sb("OHS", [128, 32], F32, ph)
                SLF = sb("SLF", [128, 64], F32, ph)
                SLFB = [Buf("slf%d" % i) for i in range(64)]
                OHr = [sb("OHr%d" % i, [128, 32], F32, ph) for i in range(8)]
                OSr = [sb("OSr%d" % i, [128, 32], F32, ph) for i in range(8)]
                p.dma("pool", wr[:], w_router.rearrange("(k p) n -> p k n", p=128), "cast", W=[wr])
                p.dma("sp", trs[:], tris, "ld", W=[trs])

                for t in range(16):
                    hd = norm_tile.a1(X1[t * 128:(t + 1) * 128, :], [dX1], GF, SHF)
                    xt_, u_ = hd
                    p.cp("pool", U2[t][:, :], u_[:], R=[u_], W=[U2[t]])
                    ut = U2Tt[t % 2]
                    norm_tile.a2(hd, ut, 0)
                    pr = psa()
                    for kc in range(8):
                        p.mm(pr[:, 0:32], ut[:, kc, :], wr[:, kc, :], start=(kc == 0), stop=(kc == 7), R=[ut, wr], W=[pr])
                    p.tt("dve", LGs[:, t, :], pr[:, 0:32], brb[:], ALU.add, R=[pr, brb], W=[LGs])
                    p.op("dve", lambda e, t=t: e.max(M8s[:, t, :], LGs[:, t, :]), R=[LGs], W=[M8s])
                    p.ts("dve", MK[:, t, :], LGs[:, t, :], M8s[:, t, 3:4], None, ALU.is_ge, R=[LGs, M8s], W=[MK])
                    p.ts("dve", NM[:], M8s[:, t, 0:1], -1.0, None, ALU.mult, R=[M8s], W=[NM])
                    p.act(EX4[:], M8s[:, t, 0:4], AF.Exp, R=[M8s, NM], W=[EX4], bias=NM[:])
                    p.op("dve", lambda e: e.tensor_reduce(S4[:], EX4[:], AX.X, ALU.add), R=[EX4], W=[S4])
                    p.op("dve", lambda e: e.reciprocal(S4[:], S4[:]), R=[S4], W=[S4])
                    p.ts("dve", CW4[:, t, :], EX4[:], S4[:], None, ALU.mult, R=[EX4, S4], W=[CW4])
                pcn = psa()
                for t in range(16):
                    p.mm(pcn[:, 0:32], onesf[:], MK[:, t, :], start=(t == 0), stop=(t == 15), R=[onesf, MK], W=[pcn])
                p.cp("dve", CNT[:], pcn[:, 0:32], R=[pcn], W=[CNT])
                p.ts("dve", PADc[:], CNT[:], 0.0, None, ALU.is_gt, R=[CNT], W=[PADc])
                for thr in (512.0, 1024.0, 1536.0):
                    p.ts("dve", TMPc[:], CNT[:], thr, None, ALU.is_gt, R=[CNT], W=[TMPc])
                    p.tt("dve", PADc[:], PADc[:], TMPc[:], ALU.add, R=[PADc, TMPc], W=[PADc])
                p.ts("dve", PADc[:], PADc[:], 512.0, None, ALU.mult, R=[PADc], W=[PADc])
                p.cp("dve", ENDa[:], PADc[:], R=[PADc], W=[ENDa])
                a_, b_ = ENDa, ENDb
                sft = 1
                while sft < 32:
                    p.tt("dve", b_[:, sft:], a_[:, sft:], a_[:, :32 - sft], ALU.add, R=[a_], W=[b_])
                    p.cp("dve", b_[:, :sft], a_[:, :sft], R=[a_], W=[b_])
                    a_, b_ = b_, a_
                    sft *= 2
                END = a_
                p.tt("dve", BASE[:], END[:], PADc[:], ALU.subtract, R=[END, PADc], W=[BASE])
                p.memset("dve", EU[:], 0.0, W=[EU])
                for e_ in range(32):
                    p.ts("dve", TU[:], uvals, END[:, e_:e_ + 1], None, ALU.is_ge, R=[mc, END], W=[TU])
                    p.tt("dve", EU[:], EU[:], TU[:], ALU.add, R=[EU, TU], W=[EU])
                p.ts("dve", EU1024[:], EU[:], 1024.0, None, ALU.mult, R=[EU], W=[EU1024])
                p.ts("dve", EU128[:], EU[:], 128.0, None, ALU.mult, R=[EU], W=[EU128])
                p.memset("dve", CWF[:], 0.0, W=CWFB)
                for t in range(16):
                    pp = psa()
                    for j in range(t):
                        p.mm(pp[:, 0:32], onesf[:], MK[:, j, :], start=(j == 0), stop=False, R=[onesf, MK], W=[pp])
                    p.mm(pp[:, 0:32], trs[:], MK[:, t, :], start=(t == 0), stop=True, R=[trs, MK], W=[pp])
                    p.tt("dve", SLT[:], pp[:, 0:32], BASE[:], ALU.add, R=[pp, BASE], W=[SLT])
                    ohs_ = [OHr[(4 * t + k) % 8] for k in range(4)]
                    oss_ = [OSr[(4 * t + k) % 8] for k in range(4)]
                    for k in range(4):
                        p.ts("dve", ohs_[k][:], LGs[:, t, :], M8s[:, t, k:k + 1], None, ALU.is_equal, R=[LGs, M8s], W=[ohs_[k]])
                    for k in range(4):
                        p.tt("dve", oss_[k][:], ohs_[k][:], SLT[:], ALU.mult, R=[ohs_[k], SLT], W=[oss_[k]])
                    for k in range(4):
                        p.op("dve", lambda e, j=4 * t + k, o_=oss_[k]: e.tensor_reduce(SLF[:, j:j + 1], o_[:], AX.X, ALU.add),
                             R=[oss_[k]], W=[SLFB[4 * t + k]])
                    for k in range(4):
                        p.stt("dve", CWF[:, t, :], ohs_[k][:], CW4[:, t, k:k + 1], CWF[:, t, :], ALU.mult, ALU.add,
                              R=[ohs_[k], CW4, CWFB[t]], W=[CWFB[t]])
                p.ts("dve", SLI[:], SLF[:], 0.0, None, ALU.add, R=SLFB, W=[SLI])
                if DEBUG:
                    p.dma("sp", DBG[:, 0:64], SLF[:], "st", R=SLFB)
                    p.dma("sp", DBG[:, 64:112], EU[:], "st", R=[EU])
                    p.dma("sp", DBG[:, 112:144], CNT[:], "st", R=[CNT])
                    p.dma("sp", DBG[:, 144:176], END[:], "st", R=[END])
                    p.dma("sp", DBG[:, 176:240], CW4[:].rearrange("p a b -> p (a b)"), "st", R=[CW4])
                for t in range(16):
                    for k in range(4):
                        j = 4 * t + k
                        p.op("pool", lambda e, t=t, j=j: e.indirect_dma_start(
                            out=Xs, out_offset=bass.IndirectOffsetOnAxis(ap=SLI[:, j:j + 1], axis=0),
                            in_=U2[t][:, :], in_offset=None),
                            R=[U2[t], SLI], W=[dXs], dma="ind")
                p.flush()

            with ExitStack() as ph:
                W1G = [[sb("w1g%d_%d" % (i, k_), [128, 1024], BF16, ph) for k_ in range(8)] for i in range(2)]
                W1L = [[sb("w1l%d_%d" % (i, k_), [128, 1024], BF16, ph) for k_ in range(8)] for i in range(2)]
                W2 = [[sb("w2_%d_%d" % (i, k_), [128, 1024], BF16, ph) for k_ in range(8)] for i in range(2)]
                XS = [sb("xs%d" % i, [128, 4, 1024], BF16, ph) for i in range(2)]
                XTu = [sb("xtu%d" % i, [128, 8, 512], BF16, ph) for i in range(2)]
                ATs = [sb("AT%d" % i, [128, 8, 512], BF16, ph) for i in range(2)]
                IDX = [sb("idx%d" % i, [128, 8], I32, ph) for i in range(3)]
                IDXB = [sb("idxb%d" % i, [128, 1], I32, ph) for i in range(3)]
                B1G = [sb("b1gu%d" % i, [128, 8], F32, ph) for i in range(3)]
                B1L = [sb("b1lu%d" % i, [128, 8], F32, ph) for i in range(3)]
                GLs = [sb("GLt%d" % i, [128, 512], F32, ph) for i in range(2)]
                SGs = [sb("SGt%d" % i, [128, 512], F32, ph) for i in range(2)]
                HBs = [sb("HBt%d" % i, [128, 512], F32, ph) for i in range(2)]
                L2s = [sb("L2t%d" % i, [128, 512], F32, ph) for i in range(2)]
                YO = [sb("yo%d" % i, [128, 1024], F32, ph) for i in range(2)]
                w1g_rows = w1g.rearrange("e k n -> (e k) n")
                w1l_rows = w1l.rearrange("e k n -> (e k) n")
                w2_rows = w2.rearrange("e k n -> (e k) n")
                cc = [0]
                yc = [0]

                bregs = {}

                def gather(dst_ap, src_rows, idx_ap, nrows, R, W):
                    def fn(e):
                        if nrows not in bregs:
                            bregs[nrows] = e.to_reg(nrows - 1)
                        return e.indirect_dma_start(
                            out=dst_ap, out_offset=None, in_=src_rows,
                            in_offset=bass.IndirectOffsetOnAxis(ap=idx_ap, axis=0),
                            bounds_check=bregs[nrows], oob_is_err=False)
                    p.op("pool", fn, R=R, W=W, dma="ind")

                ORD = []
                for g_ in range(11):
                    ORD += [3 * g_, 3 * g_ + 1, 3 * g_ + 2, 46 - g_]
                ORD += [33, 34, 35]
                assert sorted(ORD) == list(range(NU))

                def prep_w1(u):
                    if u >= NU:
                        return
                    uid = ORD[u]
                    ix, ixb = IDX[u % 3], IDXB[u % 3]
                    p.ts("dve", ix[:], rowoff, EU1024[:, uid:uid + 1], None, ALU.add, R=[mc, EU1024], W=[ix])
                    p.ts("dve", ixb[:], pidx, EU128[:, uid:uid + 1], None, ALU.add, R=[mc, EU128], W=[ixb])
                    for kc in range(8):
                        gather(W1G[u % 2][kc][:, :], w1g_rows, ix[:, kc:kc + 1], 32768, [ix], [W1G[u % 2][kc]])
                    for kc in range(8):
                        gather(W1L[u % 2][kc][:, :], w1l_rows, ix[:, kc:kc + 1], 32768, [ix], [W1L[u % 2][kc]])
                    gather(B1G[u % 3][:, :], b1g_rows, ixb[:, 0:1], 4096, [ixb], [B1G[u % 3]])
                    gather(B1L[u % 3][:, :], b1l_rows, ixb[:, 0:1], 4096, [ixb], [B1L[u % 3]])

                def prep_xs(u):
                    if u >= NU:
                        return
                    uid = ORD[u]
                    xs = XS[u % 2]
                    p.dma("sp", xs[:], Xs[uid * 512:(uid + 1) * 512, :].rearrange("(a p) n -> p a n", p=128), "ldx", R=[dXs], W=[xs])

                def prep_w2(u):
                    if u >= NU:
                        return
                    ix = IDX[u % 3]
                    for kc in range(8):
                        gather(W2[u % 2][kc][:, :], w2_rows, ix[:, kc:kc + 1], 32768, [ix], [W2[u % 2][kc]])

                def emit_mm1(u):
                    wg_, wl_ = W1G[u % 2], W1L[u % 2]
                    AT = ATs[u % 2]
                    xs, xt_ = XS[u % 2], XTu[u % 2]
                    bg, bl = B1G[u % 3], B1L[u % 3]
                    for a4 in range(4):
                        pt = pst()
                        for kc in range(8):
                            p.tr(pt[:, kc * 128:(kc + 1) * 128], xs[:, a4, kc * 128:(kc + 1) * 128], idb[:], R=[xs, idb], W=[pt])
                        p.cp("act", xt_[:, :, a4 * 128:(a4 + 1) * 128], pt[:].rearrange("p (k t) -> p k t", k=8), R=[pt], W=[xt_])
                    for c in range(8):
                        j = cc[0] % 2
                        cc[0] += 1
                        GLt, SGt, HBt, L2t = GLs[j], SGs[j], HBs[j], L2s[j]
                        pg = psa()
                        for kc in range(8):
                            p.mm(pg[:], wg_[kc][:, c * 128:(c + 1) * 128], xt_[:, kc, :],
                                 start=(kc == 0), stop=(kc == 7), R=[wg_[kc], xt_], W=[pg])
                        pl = psa()
                        for kc in range(8):
                            p.mm(pl[:], wl_[kc][:, c * 128:(c + 1) * 128], xt_[:, kc, :],
                                 start=(kc == 0), stop=(kc == 7), R=[wl_[kc], xt_], W=[pl])
                        p.ts("dve", GLt[:], pg[:], bg[:, c:c + 1], 7.0, ALU.add, ALU.min, R=[pg, bg], W=[GLt])
                        p.act(SGt[:], GLt[:], AF.Sigmoid, R=[GLt], W=[SGt], scale=1.702)
                        p.act(HBt[:], pl[:], AF.Identity, R=[pl, bl], W=[HBt], bias=bl[:, c:c + 1])
                        p.ts("dve", L2t[:], HBt[:], 7.0, -7.0, ALU.min, ALU.max, R=[HBt], W=[L2t])
                        p.tt("dve", GLt[:], GLt[:], SGt[:], ALU.mult, R=[GLt, SGt], W=[GLt])
                        p.stt("dve", AT[:, c, :], L2t[:], 1.0, GLt[:], ALU.add, ALU.mult, R=[L2t, GLt], W=[AT])

                def emit_mm2(u):
                    AT = ATs[u % 2]
                    w2_ = W2[u % 2]
                    for t4 in range(4):
                        yo = YO[yc[0] % 2]
                        yc[0] += 1
                        for hf in range(2):
                            py = psa()
                            for c in range(8):
                                p.mm(py[:], AT[:, c, t4 * 128:(t4 + 1) * 128], w2_[c][:, hf * 512:(hf + 1) * 512],
                                     start=(c == 0), stop=(c == 7), R=[AT, w2_[c]], W=[py])
                            p.cp("act", yo[:, hf * 512:(hf + 1) * 512], py[:], R=[py], W=[yo])
                        r0 = ORD[u] * 512 + t4 * 128
                        p.dma("sp", Ys[r0:r0 + 128, :], yo[:], "st", R=[yo], W=[dYs])

                nun = NU if stage >= 5 else 0
                if nun:
                    prep_xs(0)
                    prep_w1(0)
                    prep_w2(0)
                    prep_xs(1)
                    prep_w1(1)
                    emit_mm1(0)
                for u in range(nun):
                    if u + 1 < nun:
                        emit_mm1(u + 1)
                    prep_xs(u + 2)
                    emit_mm2(u)
                    prep_w1(u + 2)
                    prep_w2(u + 1)
                p.flush()

            with ExitStack() as ph:
                CWT = sb("CWTc", [32, 128], F32, ph)
                ACt = [sb("act%d" % i, [128, 1024], F32, ph) for i in range(2)]
                GB = [sb("gb%d" % i, [128, 1024], F32, ph) for i in range(8)]
                X2s = [sb("x2s%d" % i, [128, 1024], F32, ph) for i in range(2)]
                junkf = sb("junkfin", [128, 1024], BF16, ph)
                SSf = [sb("ssfin%d" % i, [128, 1], F32, ph) for i in range(2)]
                RSf = [sb("rsfin%d" % i, [128, 1], F32, ph) for i in range(2)]
                gc = [0]
                for t in range(16 if stage >= 5 else 0):
                    ac, x2, ssf, rsf = ACt[t % 2], X2s[t % 2], SSf[t % 2], RSf[t % 2]
                    p.dma("sp", x2[:], X1[t * 128:(t + 1) * 128, :], "ldx", R=[dX1], W=[x2])
                    gs = []
                    for k in range(4):
                        g_ = GB[gc[0] % 8]
                        gc[0] += 1
                        j = 4 * t + k
                        p.op("pool", lambda e, g_=g_, j=j: e.indirect_dma_start(
                            out=g_[:, :], out_offset=None, in_=Ys,
                            in_offset=bass.IndirectOffsetOnAxis(ap=SLI[:, j:j + 1], axis=0)), R=[dYs, SLI], W=[g_], dma="ind")
                        gs.append(g_)
                    pc = psa()
                    p.tr(pc[0:32, 0:128], CWF[:, t, :], idf[:], R=[CWFB[t], idf], W=[pc])
                    p.cp("act", CWT[:], pc[0:32, 0:128], R=[pc], W=[CWT])
                    for hf in range(2):
                        py = psa()
                        p.mm(py[:], CWT[:], b2s[:, hf * 512:(hf + 1) * 512], R=[CWT, b2s], W=[py])
                        p.cp("act", ac[:, hf * 512:(hf + 1) * 512], py[:], R=[py], W=[ac])
                    for k in range(4):
                        p.stt("dve", ac[:], gs[k][:], CW4[:, t, k:k + 1], ac[:], ALU.mult, ALU.add, R=[gs[k], CW4, ac], W=[ac])
                    p.tt("dve", ac[:], ac[:], GTF[:], ALU.mult, R=[ac, GTF], W=[ac])
                    p.tt("pool", x2[:], x2[:], ac[:], ALU.add, R=[x2, ac], W=[x2])
                    p.memset("pool", ssf[:], 0.0, W=[ssf])
                    p.act(junkf[:], x2[:], AF.Square, R=[x2], W=[junkf, ssf], accum_out=ssf[:])
                    p.ts("dve", rsf[:], ssf[:], 1.0 / D, EPS, ALU.mult, ALU.add, R=[ssf], W=[rsf])
                    p.act(rsf[:], rsf[:], AF.Sqrt, R=[rsf], W=[rsf])
                    p.op("dve", lambda e, rsf=rsf: e.reciprocal(rsf[:], rsf[:]), R=[rsf], W=[rsf])
                    p.stt("dve", ac[:], x2[:], rsf[:], GFIN[:], ALU.mult, ALU.mult, R=[x2, rsf, GFIN], W=[ac])
                    p.dma("sp", y[t * 128:(t + 1) * 128, :], ac[:], "st", R=[ac], W=[dY])
                p.flush()
    return nc


def _t5_buckets(dist):
    n = np.maximum(dist, 0)
    nf = np.maximum(n, 1).astype(np.float32)
    large = 16 + (np.log(nf / np.float32(16)) / np.float32(np.log(8.0)) * np.float32(16)).astype(np.int32)
    large = np.minimum(large, 31)
    return np.where(n < 16, n, large)


_NC_CACHE = {}


def kernel(x, c, w_ada, b_ada, g_mix, w_in, b_forget, sinks, rel_bias, w_proj_a, w_proj_b, w_out,
           g_ffn, w_router, b_router, w_e1, b_e1, w_e2, b_e2, g_final):
    f32 = np.float32
    A = lambda a: np.ascontiguousarray(np.asarray(a, dtype=f32))
    x2 = A(x)[0]
    xb = x2.reshape(128, 128, D)
    k = np.arange(128)[:, None]
    q = np.arange(128)[None, :]
    dist_halo = 128 + q - k
    dist_own = q - k
    rb = A(rel_bias)
    swa_bias = np.zeros((128, 2, 8, 128), f32)
    swa_bias[:, 0] = rb[_t5_buckets(dist_halo)].transpose(0, 2, 1)
    swa_bias[:, 1] = rb[_t5_buckets(dist_own)].transpose(0, 2, 1)
    swa_mask = np.zeros((128, 2, 128), f32)
    swa_mask[:, 0] = np.where((dist_halo >= 0) & (dist_halo < 128), 0.0, NEG)
    swa_mask[:, 1] = np.where((dist_own >= 0) & (dist_own < 128), 0.0, NEG)
    w1 = A(w_e1)[0]
    b1 = A(b_e1)[0]
    shared = {
        "x_all": x2,
        "c_t": A(np.asarray(c, f32).reshape(8, 128).T),
        "w_ada": A(w_ada)[0], "b_ada": A(b_ada).reshape(1, -1), "g_mix": A(g_mix).reshape(1, -1),
        "w_in": A(w_in)[0], "b_forget": A(b_forget).reshape(1, 8), "sinks": A(sinks).reshape(1, 8),
        "swa_bias": swa_bias.reshape(128, -1), "swa_mask": swa_mask.reshape(128, -1),
        "w_pa": A(w_proj_a)[0], "w_pb": A(w_proj_b)[0], "w_out": A(w_out)[0],
        "g_ffn": A(g_ffn).reshape(1, -1), "w_router": A(w_router)[0], "b_router": A(b_router).reshape(1, 32),
        "w1g": A(w1[:, :, 0::2]), "w1l": A(w1[:, :, 1::2]),
        "b1g_rows": A(b1[:, 0::2].reshape(32, 8, 128).transpose(0, 2, 1).reshape(4096, 8)),
        "b1l_rows": A(b1[:, 1::2].reshape(32, 8, 128).transpose(0, 2, 1).reshape(4096, 8)),
        "mconst": A(np.concatenate([np.tile((np.arange(48) * 512.0)[None, :], (128, 1)),
                                    (np.arange(8)[None, :] * 128.0 + np.arange(128)[:, None]),
                                    np.arange(128, dtype=f32)[:, None]], axis=1)),
        "tris": np.triu(np.ones((128, 128), f32), 1),
        "w2": A(w_e2)[0], "b2": A(b_e2)[0], "g_final": A(g_final).reshape(1, -1),
        "ident": np.eye(128, dtype=f32),
        "tri": np.triu(np.ones((128, 128), f32)),
    }
    in_maps = []
    for i in range(NCORES):
        own = xb[i::8]
        halo_ids = np.arange(16) * 8 + i - 1
        halo = np.zeros((16, 128, D), f32)
        for m_, j in enumerate(halo_ids):
            if j >= 0:
                halo[m_] = xb[j]
        kp = np.arange(128)[:, None, None]
        kb8 = np.arange(8)[None, :, None]
        qq = np.arange(128)[None, None, :]
        fm = np.where(kb8 * 128 + kp <= i * 128 + qq, BIG, NEG).astype(f32)
        meta = np.zeros((128, 24), f32)
        meta[:, i] = 1.0
        if i == 0:
            meta[:, 8] = NEG
        d = dict(shared)
        d["x_own"] = A(own.reshape(2048, D))
        d["x_halo"] = A(halo.reshape(2048, D))
        d["fmask"] = fm.reshape(128, -1)
        d["meta"] = meta
        in_maps.append(d)
    if "nc" not in _NC_CACHE:
        _NC_CACHE["nc"] = build(STAGE)
    nc = _NC_CACHE["nc"]
    res = run_bass_kernel_spmd(nc, in_maps, core_ids=list(range(NCORES)))
    if DEBUG:
        _NC_CACHE["res"] = res
    out = np.zeros((128, 128, D), f32)
    for i in range(NCORES):
        out[i::8] = res.results[i]["y"].reshape(16, 128, D)
    return out.reshape(1, S, D)
```

```python
import numpy as np
from contextlib import ExitStack
import concourse.bass as bass
import concourse.mybir as mybir
from concourse.bass_utils import run_bass_kernel_spmd

F32 = mybir.dt.float32
BF16 = mybir.dt.bfloat16
I32 = mybir.dt.int32
AF = mybir.ActivationFunctionType
ALU = mybir.AluOpType
AX = mybir.AxisListType


class Buf:
    __slots__ = ("name", "w", "r")

    def __init__(self, name=""):
        self.name = name
        self.w = None
        self.r = []


class Tn:
    def __init__(self, t, name=""):
        self.t = t
        self.b = Buf(name)

    def __getitem__(self, k):
        return self.t[k]


class Prog:
    ENG = ("pe", "act", "dve", "pool", "sp")

    def __init__(self, nc, es):
        self.nc = nc
        self.es = es
        self.sems = {e: es.enter_context(nc.semaphore("s_" + e)) for e in self.ENG}
        self.cnt = {e: 0 for e in self.ENG}
        self.chans = {}
        self.touched = set()
        self.streams = {e: [] for e in self.ENG}
        self.nphase = 0

    def chan(self, name):
        if name not in self.chans:
            self.chans[name] = [self.es.enter_context(self.nc.semaphore("c_" + name)), 0]
        return self.chans[name]

    def op(self, eng, fn, R=(), W=(), dma=None):
        deps = set()
        for b in R:
            b = b.b if isinstance(b, Tn) else b
            if b.w is not None:
                deps.add(b.w)
        for b in W:
            b = b.b if isinstance(b, Tn) else b
            if b.w is not None:
                deps.add(b.w)
            for t in b.r:
                deps.add(t)
        st = self.streams[eng]
        if dma is None:
            tok = ("e", eng, len(st))
            if eng == "pe":
                deps = {d for d in deps if not (d[0] == "e" and d[1] == "pe")}
        else:
            if eng == "pool":
                self.pool_rr = getattr(self, "pool_rr", 0) + 1
                dma = "pq%d" % (self.pool_rr % 24)
                ch = self.chan(dma)
                if ch[1] > 0:
                    deps.add(("d", dma, ch[1] - 1))
            ch = self.chan(dma)
            tok = ("d", dma, ch[1])
            ch[1] += 1
        st.append({"fn": fn, "deps": deps, "sig": False, "dma": dma})
        for b in R:
            b = b.b if isinstance(b, Tn) else b
            b.r.append(tok)
            self.touched.add(b)
        for b in W:
            b = b.b if isinstance(b, Tn) else b
            b.w = tok
            b.r = []
            self.touched.add(b)
        return tok

    def flush(self):
        nc = self.nc
        streams = self.streams
        for e in self.ENG:
            for ins in streams[e]:
                for d in ins["deps"]:
                    if d[0] == "e":
                        streams[d[1]][d[2]]["sig"] = True
        for e in self.ENG:
            for ins in reversed(streams[e]):
                if ins["dma"] is None:
                    ins["sig"] = True
                    break
        val = {}
        for e in self.ENG:
            c = self.cnt[e]
            for i, ins in enumerate(streams[e]):
                if ins["dma"] is None and ins["sig"]:
                    c += 1
                val[(e, i)] = c
            self.cnt[e] = c
        final_cnt = dict(self.cnt)
        final_ch = {k: 16 * v[1] for k, v in self.chans.items()}
        sems = self.sems
        chans = self.chans

        def emit(e, eng):
            waited = {}
            for ins in streams[e]:
                for d in sorted(ins["deps"]):
                    if d[0] == "e":
                        key = ("e", d[1])
                        v = val[(d[1], d[2])]
                        sem = sems[d[1]]
                    else:
                        key = ("d", d[1])
                        v = 16 * (d[2] + 1)
                        sem = chans[d[1]][0]
                    if waited.get(key, -1) >= v:
                        continue
                    eng.wait_ge(sem, v)
                    waited[key] = v
                bi = ins["fn"](eng)
                if ins["dma"] is not None:
                    bi.then_inc(chans[ins["dma"]][0], 16)
                elif ins["sig"]:
                    bi.then_inc(sems[e], 1)
            for o in self.ENG:
                if o != e and final_cnt[o] > 0:
                    eng.wait_ge(sems[o], final_cnt[o])
            if final_cnt[e] > 0:
                eng.wait_ge(sems[e], final_cnt[e])
            for k, v in final_ch.items():
                if v > 0:
                    eng.wait_ge(chans[k][0], v)

        with nc.Block() as blk:
            @blk.tensor
            def _(eng):
                emit("pe", eng)

            @blk.scalar
            def _(eng):
                emit("act", eng)

            @blk.vector
            def _(eng):
                emit("dve", eng)

            @blk.gpsimd
            def _(eng):
                emit("pool", eng)

            @blk.sync
            def _(eng):
                emit("sp", eng)

        self.streams = {e: [] for e in self.ENG}
        for b in self.touched:
            b.w = None
            b.r = []
        self.touched = set()
        self.nphase += 1

    def mm(self, out, lhsT, rhs, start=True, stop=True, R=(), W=()):
        return self.op("pe", lambda e: e.matmul(out, lhsT, rhs, start=start, stop=stop), R, W)

    def tr(self, out, in_, ident, R=(), W=()):
        return self.op("pe", lambda e: e.transpose(out, in_, ident), R, W)

    def act(self, out, in_, func, R=(), W=(), **kw):
        return self.op("act", lambda e: e.activation(out, in_, func, **kw), R, W)

    def ts(self, eng, out, in0, s1, s2, op0, op1=None, R=(), W=(), **kw):
        if op1 is None:
            return self.op(eng, lambda e: e.tensor_single_scalar(out, in0, s1, op0), R, W)
        return self.op(eng, lambda e: e.tensor_scalar(out, in0, s1, s2, op0, op1, **kw), R, W)

    def tt(self, eng, out, in0, in1, op, R=(), W=()):
        return self.op(eng, lambda e: e.tensor_tensor(out, in0, in1, op), R, W)

    def stt(self, eng, out, in0, scalar, in1, op0, op1, R=(), W=()):
        return self.op(eng, lambda e: e.scalar_tensor_tensor(out, in0, scalar, in1, op0, op1), R, W)

    def cp(self, eng, out, in_, R=(), W=()):
        if eng == "act":
            return self.op("act", lambda e: e.activation(out, in_, AF.Copy), R, W)
        return self.op(eng, lambda e: e.tensor_copy(out, in_), R, W)

    def memset(self, eng, ap, v, W=()):
        return self.op(eng, lambda e: e.memset(ap, v), (), W)

    def dma(self, q, out, in_, chan, R=(), W=()):
        return self.op(q, lambda e: e.dma_start(out=out, in_=in_), R, W, dma=chan)


S = 16384
D = 1024
NBLK = 128
OWN = 16
NCORES = 8
EPS = 1e-5
NEG = -30000.0
BIG = 3.0e38
STAGE = 99
DEBUG = False


def build(stage=99, dense_experts=32):
    nc = bass.Bass("TRN2", target_bir_lowering=False)

    def din(name, shape, dt=F32):
        return nc.dram_tensor(name, list(shape), dt, kind="ExternalInput").ap()

    def dscr(name, shape, dt):
        kind = "ExternalOutput" if (DEBUG and name in ("X1",)) else "Internal"
        return nc.dram_tensor(name, list(shape), dt, kind=kind).ap()

    x_all = din("x_all", [S, D])
    x_own = din("x_own", [2048, D])
    x_halo = din("x_halo", [2048, D])
    c_t = din("c_t", [128, 8])
    w_ada = din("w_ada", [D, 6 * D])
    b_ada = din("b_ada", [1, 6 * D])
    g_mix = din("g_mix", [1, D])
    w_in = din("w_in", [D, 4360])
    b_forget = din("b_forget", [1, 8])
    sinks = din("sinks", [1, 8])
    swa_bias = din("swa_bias", [128, 2 * 8 * 128])
    swa_mask = din("swa_mask", [128, 2 * 128])
    w_pa = din("w_pa", [512, D])
    w_pb = din("w_pb", [512, D])
    w_out = din("w_out", [D, D])
    g_ffn = din("g_ffn", [1, D])
    w_router = din("w_router", [D, 32])
    b_router = din("b_router", [1, 32])
    w1g = din("w1g", [32, D, D])
    w1l = din("w1l", [32, D, D])
    w2 = din("w2", [32, D, D])
    b2 = din("b2", [32, D])
    g_final = din("g_final", [1, D])
    ident = din("ident", [128, 128])
    tri = din("tri", [128, 128])
    fmask = din("fmask", [128, 8 * 128])
    meta = din("meta", [128, 24])
    mconst = din("mconst", [128, 48 + 9])
    tris = din("tris", [128, 128])
    b1g_rows = din("b1g_rows", [4096, 8])
    b1l_rows = din("b1l_rows", [4096, 8])
    y = nc.dram_tensor("y", [2048, D], F32, kind="ExternalOutput").ap()

    Ks = dscr("Ks", [8, 64, S], BF16)
    Kc = dscr("Kc", [24, S], BF16)
    Vs = dscr("Vs", [8, 128, 128, 65], BF16)
    Qs = dscr("Qs", [8, 64, 2048], BF16)
    Qc = dscr("Qc", [24, 2048], BF16)
    YA = dscr("YA", [512, 2048], BF16)
    YB = dscr("YB", [8, 64, 2048], BF16)
    X1 = dscr("X1", [2048, D], F32)
    Xs = dscr("Xs", [48 * 512, D], BF16)
    Ys = dscr("Ys", [48 * 512, D], F32)
    dXs, dYs = Buf("Xs"), Buf("Ys")
    DBG = nc.dram_tensor("DBG", [128, 256], F32, kind="ExternalOutput").ap() if DEBUG else None
    dKs, dKc, dVs, dQs, dQc, dYA, dYB, dX1, dY = (Buf(n) for n in "Ks Kc Vs Qs Qc YA YB X1 Y".split())

    with ExitStack() as es:
        p = Prog(nc, es)

        def sb(name, shape, dt, st=es):
            return Tn(st.enter_context(nc.sbuf_tensor(name, shape, dt)), name)

        def ps(name, shape, dt, st=es):
            return Tn(st.enter_context(nc.psum_tensor(name, shape, dt)), name)

        PSA = [ps("psa%d" % i, [128, 512], F32) for i in range(6)]
        PST = [ps("pst%d" % i, [128, 1024], BF16) for i in range(2)]
        rot = {"psa": 0, "pst": 0, "n": 0}

        def psa():
            rot["psa"] += 1
            return PSA[rot["psa"] % 6]

        def pst():
            rot["pst"] += 1
            return PST[rot["pst"] % 2]

        idb = sb("idb", [128, 128], BF16)
        idf = sb("idf", [128, 128], F32)
        SHF = sb("SHF", [128, D], F32)
        GF = sb("GF", [128, D], F32)
        GTF = sb("GTF", [128, D], F32)
        GFIN = sb("GFIN", [128, D], F32)
        esink = sb("esink", [128, 8], F32)
        bfb = sb("bfb", [128, 4, 8], F32)
        brb = sb("brb", [128, 32], F32)
        epsc = sb("epsc", [128, 1], F32)
        onec = sb("onec", [128, 1], F32)
        metat = sb("metat", [128, 24], F32)
        onesf = sb("onesf", [128, 128], F32)
        esm = ExitStack()
        SHM = sb("SHM", [128, D], F32, esm)
        GM = sb("GM", [128, D], F32, esm)
        GTM = sb("GTM", [128, D], F32, esm)

        with ExitStack() as ph:
            cT = sb("cT", [128, 8], F32, ph)
            cact = sb("cact", [128, 8], F32, ph)
            onesb = sb("onesb", [128, 128], BF16, ph)
            cbs = sb("cbs", [128, 8, 128], BF16, ph)
            wada = [sb("wada%d" % i, [128, 8, 1024], BF16, ph) for i in range(2)]
            bada = sb("bada", [128, 6 * D], F32, ph)
            gmb = sb("gmb", [128, D], F32, ph)
            gfb = sb("gfb", [128, D], F32, ph)
            snk = sb("snk", [128, 8], F32, ph)
            p.dma("pool", idb[:], ident, "cast", W=[idb])
            p.dma("sp", idf[:], ident, "ld", W=[idf])
            p.dma("sp", cT[:], c_t, "ld", W=[cT])
            p.dma("sp", bada[:], b_ada.partition_broadcast(128), "ld", W=[bada])
            p.dma("sp", gmb[:], g_mix.partition_broadcast(128), "ld", W=[gmb])
            p.dma("sp", gfb[:], g_ffn.partition_broadcast(128), "ld", W=[gfb])
            p.dma("sp", GFIN[:], g_final.partition_broadcast(128), "ld", W=[GFIN])
            p.dma("sp", snk[:], sinks.partition_broadcast(128), "ld", W=[snk])
            for t4 in range(4):
                p.dma("sp", bfb[:, t4, :], b_forget.partition_broadcast(128), "ld", W=[bfb])
            p.dma("sp", brb[:], b_router.partition_broadcast(128), "ld", W=[brb])
            p.dma("sp", metat[:], meta, "ld", W=[metat])
            p.memset("dve", epsc[:], EPS, W=[epsc])
            p.memset("dve", onec[:], 1.0, W=[onec])
            p.memset("dve", onesf[:], 1.0, W=[onesf])
            p.memset("dve", onesb[:], 1.0, W=[onesb])
            p.act(esink[:], snk[:], AF.Exp, R=[snk], W=[esink])
            p.act(cact[:], cT[:], AF.Silu, R=[cT], W=[cact])
            for kc in range(8):
                p.ts("dve", cbs[:, kc, :], onesb[:], cact[:, kc:kc + 1], None, ALU.mult, R=[onesb, cact], W=[cbs])
            dst = [SHM, GM, GTM, SHF, GF, GTF]
            for n in range(6):
                wa = wada[n % 2]
                p.dma("pool", wa[:], w_ada[:, n * D:(n + 1) * D].rearrange("(k p) n -> p k n", p=128), "cast", W=[wa])
                for hf in range(2):
                    pa = psa()
                    for kc in range(8):
                        p.mm(pa[:], cbs[:, kc, :], wa[:, kc, hf * 512:(hf + 1) * 512], start=(kc == 0), stop=(kc == 7),
                             R=[cbs, wa], W=[pa])
                    p.tt("dve", dst[n][:, hf * 512:(hf + 1) * 512], pa[:], bada[:, n * D + hf * 512:n * D + (hf + 1) * 512],
                         ALU.add, R=[pa, bada], W=[dst[n]])
            p.stt("dve", GM[:], GM[:], 1.0, gmb[:], ALU.add, ALU.mult, R=[GM, gmb], W=[GM])
            p.stt("dve", GF[:], GF[:], 1.0, gfb[:], ALU.add, ALU.mult, R=[GF, gfb], W=[GF])
            p.flush()

        def mk_norm(ph, tag, nx=3, nt=2, nu=2):
            XT = [sb("xt%s%d" % (tag, i), [128, D], F32, ph) for i in range(nx)]
            TMP = [sb("tmp%s%d" % (tag, i), [128, D], F32, ph) for i in range(nt)]
            U = [sb("u%s%d" % (tag, i), [128, D], BF16, ph) for i in range(nu)]
            junk = sb("junk" + tag, [128, D], BF16, ph)
            SSq = [sb("ss%s%d" % (tag, i), [128, 1], F32, ph) for i in range(3)]
            RS = [sb("rs%s%d" % (tag, i), [128, 1], F32, ph) for i in range(3)]

            def norm_a1(src, srcbuf, G, SH):
                i = rot["n"]
                rot["n"] += 1
                xt, tmp, u, ss, rs = XT[i % nx], TMP[i % nt], U[i % nu], SSq[i % 3], RS[i % 3]
                p.dma("sp", xt[:], src, "ldx", R=srcbuf, W=[xt])
                p.memset("pool", ss[:], 0.0, W=[ss])
                p.act(junk[:], xt[:], AF.Square, R=[xt], W=[junk, ss], accum_out=ss[:])
                p.ts("dve", rs[:], ss[:], 1.0 / D, EPS, ALU.mult, ALU.add, R=[ss], W=[rs])
                p.act(rs[:], rs[:], AF.Sqrt, R=[rs], W=[rs])
                p.op("dve", lambda e: e.reciprocal(rs[:], rs[:]), R=[rs], W=[rs])
                p.stt("dve", tmp[:], xt[:], rs[:], G[:], ALU.mult, ALU.mult, R=[xt, rs, G], W=[tmp])
                p.tt("pool", u[:], tmp[:], SH[:], ALU.add, R=[tmp, SH], W=[u])
                return (xt, u)

            def norm_a2(hd, uT, col0, uTbuf=None):
                xt, u = hd
                pt = pst()
                for kc in range(8):
                    p.tr(pt[:, kc * 128:(kc + 1) * 128], u[:, kc * 128:(kc + 1) * 128], idb[:], R=[u, idb], W=[pt])
                p.cp("act", uT[:, :, col0:col0 + 128], pt[:].rearrange("p (k t) -> p k t", k=8), R=[pt],
                     W=[uT if uTbuf is None else uTbuf])
                return xt

            def norm_tile(src, srcbuf, G, SH, uT, col0, uTbuf=None):
                return norm_a2(norm_a1(src, srcbuf, G, SH), uT, col0, uTbuf)
            norm_tile.a1 = norm_a1
            norm_tile.a2 = norm_a2
            return norm_tile

        with ExitStack() as ph:
            norm_tile = mk_norm(ph, "a", 3, 2, 3)
            wkv = sb("wkv", [128, 8, 1032], BF16, ph)
            UT = [sb("uTa%d" % i, [128, 8, 512], BF16, ph) for i in range(2)]
            VSB = [sb("vsb%d" % i, [128, 8, 4, 65], BF16, ph) for i in range(2)]
            KSB = [sb("ksb%d" % i, [128, 512], BF16, ph) for i in range(2)]
            LF = sb("LF", [128, 128, 8], F32, ph)
            fa = sb("fa", [128, 32], F32, ph)
            fb_ = sb("fb_", [128, 32], F32, ph)
            fc = sb("fc", [128, 32], F32, ph)
            fd = sb("fd", [128, 32], F32, ph)
            psF = PSA[5]
            p.dma("pool", wkv[:], w_in[:, 1280:2312].rearrange("(k p) n -> p k n", p=128), "cast", W=[wkv])
            for v in VSB:
                p.memset("pool", v[:], 1.0, W=[v])
            ngroups = 32 if stage >= 1 else 1
            ntile = ngroups * 4
            UTB = [[Buf('utb%d_%d' % (a_, b_)) for b_ in range(4)] for a_ in range(2)]

            hds = {}

            def partA1(T):
                hds[T] = norm_tile.a1(x_all[T * 128:(T + 1) * 128, :], [], GM, SHM)

            def partA2(T):
                g, t = divmod(T, 4)
                norm_tile.a2(hds.pop(T), UT[g % 2], t * 128, UTB[g % 2][t])

            def partB(T):
                g, t = divmod(T, 4)
                uT = UT[g % 2]
                ub = UTB[g % 2][t]
                uball = UTB[g % 2]
                vsb = VSB[g % 2]
                rot["psa"] += 1
                pv = PSA[rot["psa"] % 5]
                for kc in range(8):
                    p.mm(pv[:], uT[:, kc, t * 128:(t + 1) * 128], wkv[:, kc, 512:1024], start=(kc == 0), stop=(kc == 7),
                         R=[ub, wkv], W=[pv])
                p.cp("act", vsb[:, :, t, 0:64], pv[:].rearrange("p (h d) -> p h d", h=8), R=[pv], W=[vsb])
                for kc in range(8):
                    p.mm(psF[:, t * 8:(t + 1) * 8], uT[:, kc, t * 128:(t + 1) * 128], wkv[:, kc, 1024:1032],
                         start=(kc == 0), stop=(kc == 7), R=[ub, wkv], W=[psF])
                if t < 3:
                    return
                for cj in range(4):
                    rot["psa"] += 1
                    pk = PSA[rot["psa"] % 5]
                    for kc in range(8):
                        p.mm(pk[:], wkv[:, kc, cj * 128:(cj + 1) * 128], uT[:, kc, :], start=(kc == 0), stop=(kc == 7),
                             R=uball + [wkv], W=[pk])
                    ksb = KSB[cj % 2]
                    p.cp("act", ksb[:], pk[:], R=[pk], W=[ksb])
                    for hh in range(2):
                        p.dma("act", Ks[2 * cj + hh, :, g * 512:(g + 1) * 512], ksb[hh * 64:(hh + 1) * 64, :], "st",
                              R=[ksb], W=[dKs])
                p.dma("act", Vs[:, :, 4 * g:4 * g + 4, :].rearrange("h p b c -> p h b c"), vsb[:], "st", R=[vsb], W=[dVs])
                p.tt("dve", fa[:], psF[:, 0:32], bfb[:].rearrange("p a b -> p (a b)"), ALU.add, R=[psF, bfb], W=[fa])
                p.stt("dve", fb_[:], fa[:], -1.0, fa[:], ALU.mult, ALU.max, R=[fa], W=[fb_])
                p.act(fc[:], fb_[:], AF.Exp, R=[fb_], W=[fc], scale=-1.0)
                p.act(fc[:], fc[:], AF.Ln, R=[fc, onec], W=[fc], bias=onec[:])
                p.ts("dve", fd[:], fa[:], 0.0, None, ALU.min, R=[fa], W=[fd])
                p.tt("dve", LF[:, 4 * g:4 * g + 4, :], fd[:].rearrange("p (a b) -> p a b", a=4),
                     fc[:].rearrange("p (a b) -> p a b", a=4), ALU.subtract, R=[fd, fc], W=[LF])

            partA1(0)
            if ntile > 1:
                partA1(1)
            partA2(0)
            for T in range(ntile):
                if T + 2 < ntile:
                    partA1(T + 2)
                if T + 1 < ntile:
                    partA2(T + 1)
                partB(T)
            CI = sb("CI", [128, 1024], F32, ph)
            TOTa = sb("TOTa", [128, 128, 8], F32, ph)
            TOTb = sb("TOTb", [128, 128, 8], F32, ph)
            TOT0 = sb("TOT0", [128, 128, 8], F32, ph)
            CUM = sb("CUM", [128, 128, 8], F32, ph)
            R1 = sb("R1", [128, 1024], F32, ph)
            NN = sb("NN", [128, 128, 24], BF16, ph)
            ckT = sb("ckT", [24, S], BF16, ph)
            trif = sb("trif", [128, 128], F32, ph)
            p.dma("sp", trif[:], tri, "ld", W=[trif])
            LFf = LF[:].rearrange("p a b -> p (a b)")
            for hf in range(2):
                pa = psa()
                p.mm(pa[:], trif[:], LFf[:, hf * 512:(hf + 1) * 512], R=[trif, LF], W=[pa])
                p.cp("act", CI[:, hf * 512:(hf + 1) * 512], pa[:], R=[pa], W=[CI])
                pb = psa()
                p.mm(pb[:], onesf[:], LFf[:, hf * 512:(hf + 1) * 512], R=[onesf, LF], W=[pb])
                p.cp("act", TOT0[:].rearrange("p a b -> p (a b)")[:, hf * 512:(hf + 1) * 512], pb[:], R=[pb], W=[TOT0])
            p.cp("dve", TOTa[:], TOT0[:], R=[TOT0], W=[TOTa])
            a, b = TOTa, TOTb
            s = 1
            while s < 128:
                p.tt("dve", b[:, s:, :], a[:, s:, :], a[:, :128 - s, :], ALU.add, R=[a], W=[b])
                p.cp("dve", b[:, :s, :], a[:, :s, :], R=[a], W=[b])
                a, b = b, a
                s *= 2
            p.tt("dve", b[:], a[:], TOT0[:], ALU.subtract, R=[a, TOT0], W=[b])
            p.tt("dve", CUM[:], CI[:].rearrange("p (a b) -> p a b", b=8), b[:], ALU.add, R=[CI, b], W=[CUM])

            def split3(src3, neg, dstNN, nb):
                sgn = -1.0 if neg else 1.0
                r1 = R1[:, 0:nb * 8].rearrange("p (a b) -> p a b", b=8)
                p.ts("dve", dstNN[:, :, 0:8], src3, sgn, None, ALU.mult, R=[CUM, OCt], W=[dstNN])
                p.stt("dve", r1, src3, sgn, dstNN[:, :, 0:8], ALU.mult, ALU.subtract, R=[CUM, OCt, dstNN], W=[R1])
                p.cp("dve", dstNN[:, :, 8:16], r1, R=[R1], W=[dstNN])
                p.tt("dve", r1, r1, dstNN[:, :, 8:16], ALU.subtract, R=[R1, dstNN], W=[R1])
                p.cp("dve", dstNN[:, :, 16:24], r1, R=[R1], W=[dstNN])

            OCt = sb("OCt", [128, 16, 8], F32, ph)
            QN = sb("QN", [128, 16, 24], BF16, ph)
            cqT = sb("cqT", [24, 2048], BF16, ph)
            split3(CUM[:], True, NN, 128)
            for b8 in range(16):
                pt = pst()
                for j in range(8):
                    blk = b8 * 8 + j
                    p.tr(pt[0:24, j * 128:(j + 1) * 128], NN[:, blk, :], idb[:], R=[NN, idb], W=[pt])
                p.cp("act", ckT[:, b8 * 1024:(b8 + 1) * 1024], pt[0:24, :], R=[pt], W=[ckT])
            p.dma("sp", Kc, ckT[:], "st", R=[ckT], W=[dKc])
            CUM4 = CUM[:].rearrange("p (m r) h -> p m r h", r=8)
            p.ts("dve", OCt[:], CUM4[:, :, 0, :], metat[:, 0:1], None, ALU.mult, R=[CUM, metat], W=[OCt])
            for r in range(1, 8):
                p.stt("dve", OCt[:], CUM4[:, :, r, :], metat[:, r:r + 1], OCt[:], ALU.mult, ALU.add, R=[CUM, metat, OCt], W=[OCt])
            split3(OCt[:], False, QN, 16)
            for b8 in range(2):
                pt = pst()
                for j in range(8):
                    p.tr(pt[0:24, j * 128:(j + 1) * 128], QN[:, b8 * 8 + j, :], idb[:], R=[QN, idb], W=[pt])
                p.cp("act", cqT[:, b8 * 1024:(b8 + 1) * 1024], pt[0:24, :], R=[pt], W=[cqT])
            p.dma("sp", Qc, cqT[:], "st", R=[cqT], W=[dQc])
            p.flush()

        with ExitStack() as ph:
            norm_tile = mk_norm(ph, "c")
            wq = sb("wq", [128, 8, 1280], BF16, ph)
            UTo = sb("uTo", [128, 8, 512], BF16, ph)
            UTh = sb("uTh", [128, 8, 512], BF16, ph)
            QSB = [sb("qsb%d" % i, [128, 512], BF16, ph) for i in range(2)]
            QA = sb("QA", [64, 8, 512], BF16, ph)
            KA = sb("KA", [64, 2, 2, 512], BF16, ph)
            VA = sb("VA", [128, 2, 4, 2, 65], BF16, ph)
            BM = sb("BM", [128, 2, 8, 128], F32, ph)
            MK = sb("MK", [128, 2, 128], F32, ph)
            SBI = [sb("sbi%d" % i, [128, 512], F32, ph) for i in range(2)]
            PTs = [sb("pts%d" % i, [128, 512], BF16, ph) for i in range(2)]
            den = sb("den", [128, 8], F32, ph)
            yat = sb("yat", [128, 512], BF16, ph)
            YAT = sb("YATt", [128, 4, 128], BF16, ph)
            p.dma("pool", wq[:], w_in[:, 0:1280].rearrange("(k p) n -> p k n", p=128), "cast", W=[wq])
            p.dma("sp", BM[:].rearrange("p a h q -> p (a h q)"), swa_bias, "ld", W=[BM])
            p.dma("sp", MK[:].rearrange("p a q -> p (a q)"), swa_mask, "ld", W=[MK])
            for hf in range(2):
                for h in range(8):
                    p.tt("dve", BM[:, hf, h, :], BM[:, hf, h, :], MK[:, hf, :], ALU.add, R=[BM, MK], W=[BM])
            p.memset("pool", VA[:], 1.0, W=[VA])
            for gq in range(4 if stage >= 2 else 0):
                for t in range(4):
                    m = 4 * gq + t
                    norm_tile(x_own[m * 128:(m + 1) * 128, :], [], GM, SHM, UTo, t * 128)
                    norm_tile(x_halo[m * 128:(m + 1) * 128, :], [], GM, SHM, UTh, t * 128)
                for cj in range(4):
                    pq = psa()
                    for kc in range(8):
                        p.mm(pq[:], wq[:, kc, 768 + cj * 128:768 + (cj + 1) * 128], UTo[:, kc, :], start=(kc == 0), stop=(kc == 7),
                             R=[wq, UTo], W=[pq])
                    qsb = QSB[cj % 2]
                    p.act(qsb[:], pq[:], AF.Copy, R=[pq], W=[qsb], scale=0.125)
                    for hh in range(2):
                        p.dma("sp", Qs[2 * cj + hh, :, gq * 512:(gq + 1) * 512], qsb[hh * 64:(hh + 1) * 64, :], "st",
                              R=[qsb], W=[dQs])
                for h in range(8):
                    pq = psa()
                    for kc in range(8):
                        p.mm(pq[0:64, :], wq[:, kc, h * 64:(h + 1) * 64], UTo[:, kc, :], start=(kc == 0), stop=(kc == 7),
                             R=[wq, UTo], W=[pq])
                    p.act(QA[:, h, :], pq[0:64, :], AF.Copy, R=[pq], W=[QA], scale=0.125)
                for si, UTs in enumerate((UTh, UTo)):
                    for kvh in range(2):
                        pq = psa()
                        for kc in range(8):
                            p.mm(pq[0:64, :], wq[:, kc, 512 + kvh * 64:512 + (kvh + 1) * 64], UTs[:, kc, :],
                                 start=(kc == 0), stop=(kc == 7), R=[wq, UTs], W=[pq])
                        p.cp("act", KA[:, si, kvh, :], pq[0:64, :], R=[pq], W=[KA])
                    for t in range(4):
                        pq = psa()
                        for kc in range(8):
                            p.mm(pq[:, 0:128], UTs[:, kc, t * 128:(t + 1) * 128], wq[:, kc, 640:768],
                                 start=(kc == 0), stop=(kc == 7), R=[wq, UTs], W=[pq])
                        p.cp("dve", VA[:, si, t, :, 0:64], pq[:, 0:128].rearrange("p (a d) -> p a d", a=2), R=[pq], W=[VA])
                for t in range(4):
                    m = 4 * gq + t
                    pos = [psa(), psa()]
                    for kvh in range(2):
                        po = pos[kvh]
                        pTs = []
                        for si in range(2):
                            pS = psa()
                            p.mm(pS[:], KA[:, si, kvh, t * 128:(t + 1) * 128], QA[:, kvh * 4:(kvh + 1) * 4, t * 128:(t + 1) * 128],
                                 R=[KA, QA], W=[pS])
                            sbi = SBI[si]
                            p.tt("dve", sbi[:], pS[:], BM[:, si, kvh * 4:(kvh + 1) * 4, :].rearrange("p h q -> p (h q)"),
                                 ALU.add, R=[pS, BM], W=[sbi])
                            pT = PTs[si]
                            if si == 0:
                                p.act(pT[:], sbi[:], AF.Exp, R=[sbi, metat], W=[pT], bias=metat[:, 8 + m:9 + m])
                            else:
                                p.act(pT[:], sbi[:], AF.Exp, R=[sbi], W=[pT])
                            pTs.append(pT)
                        for g4 in range(4):
                            for si in range(2):
                                p.mm(po[:, g4 * 65:(g4 + 1) * 65], pTs[si][:, g4 * 128:(g4 + 1) * 128], VA[:, si, t, kvh, :],
                                     start=(si == 0), stop=(si == 1), R=[pTs[si], VA], W=[po])
                    for kvh in range(2):
                        po = pos[kvh]
                        po3 = po[:, 0:260].rearrange("p (g c) -> p g c", c=65)
                        p.tt("dve", den[:, kvh * 4:(kvh + 1) * 4], po3[:, :, 64], esink[:, kvh * 4:(kvh + 1) * 4], ALU.add,
                             R=[po, esink], W=[den])
                        p.op("dve", lambda e, k=kvh: e.reciprocal(den[:, k * 4:(k + 1) * 4], den[:, k * 4:(k + 1) * 4]), R=[den], W=[den])
                        for g4 in range(4):
                            h = kvh * 4 + g4
                            p.ts("dve", yat[:, h * 64:(h + 1) * 64], po[:, g4 * 65:g4 * 65 + 64], den[:, h:h + 1], None, ALU.mult,
                                 R=[po, den], W=[yat])
                    pt = pst()
                    for c4 in range(4):
                        p.tr(pt[:, c4 * 128:(c4 + 1) * 128], yat[:, c4 * 128:(c4 + 1) * 128], idb[:], R=[yat, idb], W=[pt])
                    p.cp("act", YAT[:], pt[:, 0:512].rearrange("p (c t) -> p c t", c=4), R=[pt], W=[YAT])
                    p.dma("sp", YA[:, m * 128:(m + 1) * 128].rearrange("(c p) t -> p c t", p=128), YAT[:], "st", R=[YAT], W=[dYA])
            p.flush()

        with ExitStack() as ph:
            KAUG = [sb("kaug%d" % i, [70, S], BF16, ph) for i in range(2)]
            VH = [sb("vh%d" % i, [128, 128, 65], BF16, ph) for i in range(2)]
            QH = [sb("qh%d" % i, [70, 2048], BF16, ph) for i in range(2)]
            PT = [sb("pt%d" % i, [128, 512], BF16, ph) for i in range(3)]
            MSK = [sb("msk%d" % i, [128, 512], F32, ph) for i in range(2)]
            FM = sb("FM", [128, 8, 512], F32, ph)
            LR = sb("LR", [65, 512], F32, ph)
            BCS = sb("BCS", [64, 512], F32, ph)
            YBS = [sb("ybs%d" % i, [64, 512], BF16, ph) for i in range(2)]
            p.memset("pool", FM[:], BIG, W=[FM])
            p.dma("sp", FM[:, :, 0:128], fmask.rearrange("p (a q) -> p a q", a=8), "ld", W=[FM])
            for i in range(2):
                p.memset("pool", KAUG[i][64:70, :], 1.0, W=[KAUG[i]])
                p.memset("pool", QH[i][64:70, :], 1.0, W=[QH[i]])
            Kc3 = Kc.rearrange("(x h) t -> x h t", x=3)
            Qc3 = Qc.rearrange("(x h) t -> x h t", x=3)
            PSS = PSA[0:3]
            PSO = PSA[3:5]
            psBC = PSA[5]
            its = []
            gi = 0
            for h in range(8 if stage >= 3 else 0):
                for g in range(4):
                    q0 = g * 512
                    lst = []
                    for kb in range(32 * g):
                        lst.append(dict(h=h, kb=kb, n=512, c0=0, q0=q0, masked=False, kb8=0, gi=gi))
                    for r in range(4):
                        for kb8 in range(8):
                            lst.append(dict(h=h, kb=32 * g + 8 * r + kb8, n=(4 - r) * 128, c0=r * 128, q0=q0, masked=True,
                                            kb8=kb8, gi=gi))
                    lst[0]["first"] = True
                    lst[-1]["last"] = True
                    its.extend(lst)
                    gi += 1
            loaded = set()

            def load_head(h):
                if h in loaded or h >= 8:
                    return
                loaded.add(h)
                ka, vh, qh = KAUG[h % 2], VH[h % 2], QH[h % 2]
                p.dma("sp", ka[0:64, :], Ks[h], "ldk", R=[dKs], W=[ka])
                p.dma("sp", ka[67:70, :], Kc3[:, h, :], "ldk", R=[dKc], W=[ka])
                p.dma("sp", vh[:], Vs[h], "ldk", R=[dVs], W=[vh])
                p.dma("sp", qh[0:64, :], Qs[h], "ldk", R=[dQs], W=[qh])
                p.dma("sp", qh[64:67, :], Qc3[:, h, :], "ldk", R=[dQc], W=[qh])

            def emitS(i):
                d = its[i]
                h = d["h"]
                load_head(h)
                ka, qh = KAUG[h % 2], QH[h % 2]
                pS, pT = PSS[i % 3], PT[i % 3]
                n, c0, q0, kb = d["n"], d["c0"], d["q0"], d["kb"]
                p.mm(pS[:, 0:n], ka[0:70, kb * 128:(kb + 1) * 128], qh[0:70, q0 + c0:q0 + 512], R=[ka, qh], W=[pS])
                if d["masked"]:
                    mk = MSK[i % 2]
                    p.tt("dve", mk[:, 0:n], pS[:, 0:n], FM[:, d["kb8"], 0:n], ALU.min, R=[pS, FM], W=[mk])
                    p.act(pT[:, 0:n], mk[:, 0:n], AF.Exp, R=[mk], W=[pT])
                else:
                    p.act(pT[:], pS[:], AF.Exp, R=[pS], W=[pT])

            def emitPV(i):
                d = its[i]
                h = d["h"]
                vh = VH[h % 2]
                po = PSO[d["gi"] % 2]
                pT = PT[i % 3]
                n, c0, q0, kb = d["n"], d["c0"], d["q0"], d["kb"]
                p.mm(po[0:65, c0:512], vh[:, kb, :], pT[:, 0:n], start=bool(d.get("first")), stop=bool(d.get("last")),
                     R=[vh, pT], W=[po])
                if d.get("last"):
                    if q0 == 0:
                        load_head(h + 1)
                    p.cp("act", LR[64:65, :], po[64:65, :], R=[po], W=[LR])
                    p.op("dve", lambda e: e.reciprocal(LR[64:65, :], LR[64:65, :]), R=[LR], W=[LR])
                    p.mm(psBC[0:64, :], onesf[64:65, 0:64], LR[64:65, :], R=[onesf, LR], W=[psBC])
                    p.cp("act", BCS[:], psBC[0:64, :], R=[psBC], W=[BCS])
                    ybs = YBS[d["gi"] % 2]
                    p.tt("dve", ybs[:], po[0:64, :], BCS[:], ALU.mult, R=[po, BCS], W=[ybs])
                    p.dma("sp", YB[h, :, q0:q0 + 512], ybs[:], "st", R=[ybs], W=[dYB])

            LA = 2
            for i in range(len(its) + LA):
                if i < len(its):
                    emitS(i)
                if i - LA >= 0:
                    emitPV(i - LA)
            p.flush()

        with ExitStack() as ph:
            norm_tile = mk_norm(ph, "e")
            wg = sb("wg", [128, 8, 2048], BF16, ph)
            wpa = sb("wpa", [128, 4, 1024], BF16, ph)
            wpb = sb("wpb", [64, 8, 1024], BF16, ph)
            wo = sb("wo", [128, 8, 1024], BF16, ph)
            UTb = sb("uTb", [128, 8, 128], BF16, ph)
            SG = sb("SG", [128, 2048], F32, ph)
            yaTb = sb("yaTb", [128, 4, 128], BF16, ph)
            ybTb = sb("ybTb", [64, 8, 128], BF16, ph)
            T1 = sb("T1", [128, 512], F32, ph)
            T2 = sb("T2", [128, 512], F32, ph)
            MG = sb("MG", [128, 1024], BF16, ph)
            MGT = sb("MGT", [128, 8, 128], BF16, ph)
            T3 = sb("T3", [128, 1024], F32, ph)
            X1t = [sb("x1t%d" % i, [128, 1024], F32, ph) for i in range(2)]
            p.dma("pool", wg[:], w_in[:, 2312:4360].rearrange("(k p) n -> p k n", p=128), "cast", W=[wg])
            p.dma("pool", wpa[:], w_pa.rearrange("(c p) n -> p c n", p=128), "cast", W=[wpa])
            p.dma("pool", wpb[:], w_pb.rearrange("(h p) n -> p h n", p=64), "cast", W=[wpb])
            p.dma("pool", wo[:], w_out.rearrange("(k p) n -> p k n", p=128), "cast", W=[wo])
            ZT = sb("ZT", [128, 4, 1024], BF16, ph)
            p.memset("pool", ZT[:], 0.0, W=[ZT])
            for u_ in range(48):
                p.dma("sp", Xs[u_ * 512:(u_ + 1) * 512, :].rearrange("(a p) n -> p a n", p=128), ZT[:], "st", R=[ZT], W=[dXs])
            UTbs = [UTb, sb("uTb2", [128, 8, 128], BF16, ph)]
            SGs_ = [SG, sb("SG2", [128, 2048], F32, ph)]
            yaTbs = [yaTb, sb("yaTb2", [128, 4, 128], BF16, ph)]
            ybTbs = [ybTb, sb("ybTb2", [64, 8, 128], BF16, ph)]
            xts = {}

            def stage1(m):
                UTb_, SG_, ya_, yb_ = UTbs[m % 2], SGs_[m % 2], yaTbs[m % 2], ybTbs[m % 2]
                xts[m] = norm_tile(x_own[m * 128:(m + 1) * 128, :], [], GM, SHM, UTb_, 0)
                p.dma("sp", ya_[:], YA[:, m * 128:(m + 1) * 128].rearrange("(c p) t -> p c t", p=128), "ldy", R=[dYA], W=[ya_])
                p.dma("sp", yb_[:], YB[:, :, m * 128:(m + 1) * 128].rearrange("h d t -> d h t"), "ldy", R=[dYB], W=[yb_])
                for j in range(4):
                    pg = psa()
                    for kc in range(8):
                        p.mm(pg[:], UTb_[:, kc, :], wg[:, kc, j * 512:(j + 1) * 512], start=(kc == 0), stop=(kc == 7),
                             R=[UTb_, wg], W=[pg])
                    p.act(SG_[:, j * 512:(j + 1) * 512], pg[:], AF.Sigmoid, R=[pg], W=[SG_])

            def stage2(m):
                SG_, ya_, yb_ = SGs_[m % 2], yaTbs[m % 2], ybTbs[m % 2]
                xt = xts.pop(m)
                for hf in range(2):
                    pa = psa()
                    for c4 in range(4):
                        p.mm(pa[:], ya_[:, c4, :], wpa[:, c4, hf * 512:(hf + 1) * 512], start=(c4 == 0), stop=(c4 == 3),
                             R=[ya_, wpa], W=[pa])
                    pb = psa()
                    for h in range(8):
                        p.mm(pb[:], yb_[:, h, :], wpb[:, h, hf * 512:(hf + 1) * 512], start=(h == 0), stop=(h == 7),
                             R=[yb_, wpb], W=[pb])
                    p.tt("dve", T1[:], pa[:], SG_[:, hf * 512:(hf + 1) * 512], ALU.mult, R=[pa, SG_], W=[T1])
                    p.tt("dve", T2[:], pb[:], SG_[:, 1024 + hf * 512:1024 + (hf + 1) * 512], ALU.mult, R=[pb, SG_], W=[T2])
                    p.tt("pool", MG[:, hf * 512:(hf + 1) * 512], T1[:], T2[:], ALU.add, R=[T1, T2], W=[MG])
                pt = pst()
                for kc in range(8):
                    p.tr(pt[:, kc * 128:(kc + 1) * 128], MG[:, kc * 128:(kc + 1) * 128], idb[:], R=[MG, idb], W=[pt])
                p.cp("act", MGT[:], pt[:].rearrange("p (k t) -> p k t", k=8), R=[pt], W=[MGT])
                x1 = X1t[m % 2]
                for hf in range(2):
                    pw = psa()
                    for kc in range(8):
                        p.mm(pw[:], MGT[:, kc, :], wo[:, kc, hf * 512:(hf + 1) * 512], start=(kc == 0), stop=(kc == 7),
                             R=[MGT, wo], W=[pw])
                    p.tt("dve", T3[:, hf * 512:(hf + 1) * 512], pw[:], GTM[:, hf * 512:(hf + 1) * 512], ALU.mult, R=[pw, GTM], W=[T3])
                p.tt("pool", x1[:], T3[:], xt[:], ALU.add, R=[T3, xt], W=[x1])
                p.dma("sp", X1[m * 128:(m + 1) * 128, :], x1[:], "st", R=[x1], W=[dX1])

            nblk = 16 if stage >= 4 else 0
            if nblk:
                stage1(0)
            for m in range(nblk):
                if m + 1 < nblk:
                    stage1(m + 1)
                stage2(m)
            p.flush()

        esm.close()
        NU = 47
        NSLOT = NU * 512
        with ExitStack() as phF:
            CWF = sb("CWF", [128, 16, 32], F32, phF)
            CWFB = [Buf("cwf%d" % i) for i in range(16)]
            CW4 = sb("CW4", [128, 16, 4], F32, phF)
            SLI = sb("SLI", [128, 64], I32, phF)
            EU = sb("EU", [128, NU], F32, phF)
            EU1024 = sb("EU1024", [128, NU], F32, phF)
            EU128 = sb("EU128", [128, NU], F32, phF)
            mc = sb("mc", [128, 57], F32, phF)
            b2s = sb("b2s", [32, 1024], F32, phF)
            p.dma("sp", mc[:], mconst, "ld", W=[mc])
            p.dma("sp", b2s[:], b2, "ld", W=[b2s])
            uvals = mc[:, 0:NU]
            rowoff = mc[:, 48:56]
            pidx = mc[:, 56:57]
            with ExitStack() as ph:
                norm_tile = mk_norm(ph, "f", 2, 1, 1)
                U2 = [sb("U2_%d" % i, [128, 1024], BF16, ph) for i in range(16)]
                U2Tt = [sb("U2Tt%d" % i, [128, 8, 128], BF16, ph) for i in range(2)]
                wr = sb("wr", [128, 8, 32], BF16, ph)
                trs = sb("trs", [128, 128], F32, ph)
                LGs = sb("LGs", [128, 16, 32], F32, ph)
                M8s = sb("M8s", [128, 16, 8], F32, ph)
                MK = sb("MKrt", [128, 16, 32], F32, ph)
                NM = sb("NM", [128, 1], F32, ph)
                EX4 = sb("EX4", [128, 4], F32, ph)
                S4 = sb("S4", [128, 1], F32, ph)
                CNT = sb("CNT", [128, 32], F32, ph)
                PADc = sb("PADc", [128, 32], F32, ph)
                TMPc = sb("TMPc", [128, 32], F32, ph)
                ENDa = sb("ENDa", [128, 32], F32, ph)
                ENDb = sb("ENDb", [128, 32], F32, ph)
                BASE = sb("BASE", [128, 32], F32, ph)
                TU = sb("TU", [128, NU], F32, ph)
                SLT = sb("SLT", [128, 32], F32, ph)
                OH = sb("OH", [128, 32], F32, ph)
                OHS = sb("OHS", [128, 32], F32, ph)
                SLF = sb("SLF", [128, 64], F32, ph)
                SLFB = [Buf("slf%d" % i) for i in range(64)]
                OHr = [sb("OHr%d" % i, [128, 32], F32, ph) for i in range(8)]
                OSr = [sb("OSr%d" % i, [128, 32], F32, ph) for i in range(8)]
                p.dma("pool", wr[:], w_router.rearrange("(k p) n -> p k n", p=128), "cast", W=[wr])
                p.dma("sp", trs[:], tris, "ld", W=[trs])

                for t in range(16):
                    hd = norm_tile.a1(X1[t * 128:(t + 1) * 128, :], [dX1], GF, SHF)
                    xt_, u_ = hd
                    p.cp("pool", U2[t][:, :], u_[:], R=[u_], W=[U2[t]])
                    ut = U2Tt[t % 2]
                    norm_tile.a2(hd, ut, 0)
                    pr = psa()
                    for kc in range(8):
                        p.mm(pr[:, 0:32], ut[:, kc, :], wr[:, kc, :], start=(kc == 0), stop=(kc == 7), R=[ut, wr], W=[pr])
                    p.tt("dve", LGs[:, t, :], pr[:, 0:32], brb[:], ALU.add, R=[pr, brb], W=[LGs])
                    p.op("dve", lambda e, t=t: e.max(M8s[:, t, :], LGs[:, t, :]), R=[LGs], W=[M8s])
                    p.ts("dve", MK[:, t, :], LGs[:, t, :], M8s[:, t, 3:4], None, ALU.is_ge, R=[LGs, M8s], W=[MK])
                    p.ts("dve", NM[:], M8s[:, t, 0:1], -1.0, None, ALU.mult, R=[M8s], W=[NM])
                    p.act(EX4[:], M8s[:, t, 0:4], AF.Exp, R=[M8s, NM], W=[EX4], bias=NM[:])
                    p.op("dve", lambda e: e.tensor_reduce(S4[:], EX4[:], AX.X, ALU.add), R=[EX4], W=[S4])
                    p.op("dve", lambda e: e.reciprocal(S4[:], S4[:]), R=[S4], W=[S4])
                    p.ts("dve", CW4[:, t, :], EX4[:], S4[:], None, ALU.mult, R=[EX4, S4], W=[CW4])
                pcn = psa()
                for t in range(16):
                    p.mm(pcn[:, 0:32], onesf[:], MK[:, t, :], start=(t == 0), stop=(t == 15), R=[onesf, MK], W=[pcn])
                p.cp("dve", CNT[:], pcn[:, 0:32], R=[pcn], W=[CNT])
                p.ts("dve", PADc[:], CNT[:], 0.0, None, ALU.is_gt, R=[CNT], W=[PADc])
                for thr in (512.0, 1024.0, 1536.0):
                    p.ts("dve", TMPc[:], CNT[:], thr, None, ALU.is_gt, R=[CNT], W=[TMPc])
                    p.tt("dve", PADc[:], PADc[:], TMPc[:], ALU.add, R=[PADc, TMPc], W=[PADc])
                p.ts("dve", PADc[:], PADc[:], 512.0, None, ALU.mult, R=[PADc], W=[PADc])
                p.cp("dve", ENDa[:], PADc[:], R=[PADc], W=[ENDa])
                a_, b_ = ENDa, ENDb
                sft = 1
                while sft < 32:
                    p.tt("dve", b_[:, sft:], a_[:, sft:], a_[:, :32 - sft], ALU.add, R=[a_], W=[b_])
                    p.cp("dve", b_[:, :sft], a_[:, :sft], R=[a_], W=[b_])
                    a_, b_ = b_, a_
                    sft *= 2
                END = a_
                p.tt("dve", BASE[:], END[:], PADc[:], ALU.subtract, R=[END, PADc], W=[BASE])
                p.memset("dve", EU[:], 0.0, W=[EU])
                for e_ in range(32):
                    p.ts("dve", TU[:], uvals, END[:, e_:e_ + 1], None, ALU.is_ge, R=[mc, END], W=[TU])
                    p.tt("dve", EU[:], EU[:], TU[:], ALU.add, R=[EU, TU], W=[EU])
                p.ts("dve", EU1024[:], EU[:], 1024.0, None, ALU.mult, R=[EU], W=[EU1024])
                p.ts("dve", EU128[:], EU[:], 128.0, None, ALU.mult, R=[EU], W=[EU128])
                p.memset("dve", CWF[:], 0.0, W=CWFB)
                for t in range(16):
                    pp = psa()
                    for j in range(t):
                        p.mm(pp[:, 0:32], onesf[:], MK[:, j, :], start=(j == 0), stop=False, R=[onesf, MK], W=[pp])
                    p.mm(pp[:, 0:32], trs[:], MK[:, t, :], start=(t == 0), stop=True, R=[trs, MK], W=[pp])
                    p.tt("dve", SLT[:], pp[:, 0:32], BASE[:], ALU.add, R=[pp, BASE], W=[SLT])
                    ohs_ = [OHr[(4 * t + k) % 8] for k in range(4)]
                    oss_ = [OSr[(4 * t + k) % 8] for k in range(4)]
                    for k in range(4):
                        p.ts("dve", ohs_[k][:], LGs[:, t, :], M8s[:, t, k:k + 1], None, ALU.is_equal, R=[LGs, M8s], W=[ohs_[k]])
                    for k in range(4):
                        p.tt("dve", oss_[k][:], ohs_[k][:], SLT[:], ALU.mult, R=[ohs_[k], SLT], W=[oss_[k]])
                    for k in range(4):
                        p.op("dve", lambda e, j=4 * t + k, o_=oss_[k]: e.tensor_reduce(SLF[:, j:j + 1], o_[:], AX.X, ALU.add),
                             R=[oss_[k]], W=[SLFB[4 * t + k]])
                    for k in range(4):
                        p.stt("dve", CWF[:, t, :], ohs_[k][:], CW4[:, t, k:k + 1], CWF[:, t, :], ALU.mult, ALU.add,
                              R=[ohs_[k], CW4, CWFB[t]], W=[CWFB[t]])
                p.ts("dve", SLI[:], SLF[:], 0.0, None, ALU.add, R=SLFB, W=[SLI])
                if DEBUG:
                    p.dma("sp", DBG[:, 0:64], SLF[:], "st", R=SLFB)
                    p.dma("sp", DBG[:, 64:112], EU[:], "st", R=[EU])
                    p.dma("sp", DBG[:, 112:144], CNT[:], "st", R=[CNT])
                    p.dma("sp", DBG[:, 144:176], END[:], "st", R=[END])
                    p.dma("sp", DBG[:, 176:240], CW4[:].rearrange("p a b -> p (a b)"), "st", R=[CW4])
                for t in range(16):
                    for k in range(4):
                        j = 4 * t + k
                        p.op("pool", lambda e, t=t, j=j: e.indirect_dma_start(
                            out=Xs, out_offset=bass.IndirectOffsetOnAxis(ap=SLI[:, j:j + 1], axis=0),
                            in_=U2[t][:, :], in_offset=None),
                            R=[U2[t], SLI], W=[dXs], dma="ind")
                p.flush()

            with ExitStack() as ph:
                W1G = [[sb("w1g%d_%d" % (i, k_), [128, 1024], BF16, ph) for k_ in range(8)] for i in range(2)]
                W1L = [[sb("w1l%d_%d" % (i, k_), [128, 1024], BF16, ph) for k_ in range(8)] for i in range(2)]
                W2 = [[sb("w2_%d_%d" % (i, k_), [128, 1024], BF16, ph) for k_ in range(8)] for i in range(2)]
                XS = [sb("xs%d" % i, [128, 4, 1024], BF16, ph) for i in range(2)]
                XTu = [sb("xtu%d" % i, [128, 8, 512], BF16, ph) for i in range(2)]
                ATs = [sb("AT%d" % i, [128, 8, 512], BF16, ph) for i in range(2)]
                IDX = [sb("idx%d" % i, [128, 8], I32, ph) for i in range(3)]
                IDXB = [sb("idxb%d" % i, [128, 1], I32, ph) for i in range(3)]
                B1G = [sb("b1gu%d" % i, [128, 8], F32, ph) for i in range(3)]
                B1L = [sb("b1lu%d" % i, [128, 8], F32, ph) for i in range(3)]
                GLs = [sb("GLt%d" % i, [128, 512], F32, ph) for i in range(2)]
                SGs = [sb("SGt%d" % i, [128, 512], F32, ph) for i in range(2)]
                HBs = [sb("HBt%d" % i, [128, 512], F32, ph) for i in range(2)]
                L2s = [sb("L2t%d" % i, [128, 512], F32, ph) for i in range(2)]
                YO = [sb("yo%d" % i, [128, 1024], F32, ph) for i in range(2)]
                w1g_rows = w1g.rearrange("e k n -> (e k) n")
                w1l_rows = w1l.rearrange("e k n -> (e k) n")
                w2_rows = w2.rearrange("e k n -> (e k) n")
                cc = [0]
                yc = [0]

                bregs = {}

                def gather(dst_ap, src_rows, idx_ap, nrows, R, W):
                    def fn(e):
                        if nrows not in bregs:
                            bregs[nrows] = e.to_reg(nrows - 1)
                        return e.indirect_dma_start(
                            out=dst_ap, out_offset=None, in_=src_rows,
                            in_offset=bass.IndirectOffsetOnAxis(ap=idx_ap, axis=0),
                            bounds_check=bregs[nrows], oob_is_err=False)
                    p.op("pool", fn, R=R, W=W, dma="ind")

                ORD = []
                for g_ in range(11):
                    ORD += [3 * g_, 3 * g_ + 1, 3 * g_ + 2, 46 - g_]
                ORD += [33, 34, 35]
                assert sorted(ORD) == list(range(NU))

                def prep_w1(u):
                    if u >= NU:
                        return
                    uid = ORD[u]
                    ix, ixb = IDX[u % 3], IDXB[u % 3]
                    p.ts("dve", ix[:], rowoff, EU1024[:, uid:uid + 1], None, ALU.add, R=[mc, EU1024], W=[ix])
                    p.ts("dve", ixb[:], pidx, EU128[:, uid:uid + 1], None, ALU.add, R=[mc, EU128], W=[ixb])
                    for kc in range(8):
                        gather(W1G[u % 2][kc][:, :], w1g_rows, ix[:, kc:kc + 1], 32768, [ix], [W1G[u % 2][kc]])
                    for kc in range(8):
                        gather(W1L[u % 2][kc][:, :], w1l_rows, ix[:, kc:kc + 1], 32768, [ix], [W1L[u % 2][kc]])
                    gather(B1G[u % 3][:, :], b1g_rows, ixb[:, 0:1], 4096, [ixb], [B1G[u % 3]])
                    gather(B1L[u % 3][:, :], b1l_rows, ixb[:, 0:1], 4096, [ixb], [B1L[u % 3]])

                def prep_xs(u):
                    if u >= NU:
                        return
                    uid = ORD[u]
                    xs = XS[u % 2]
                    p.dma("sp", xs[:], Xs[uid * 512:(uid + 1) * 512, :].rearrange("(a p) n -> p a n", p=128), "ldx", R=[dXs], W=[xs])

                def prep_w2(u):
                    if u >= NU:
                        return
                    ix = IDX[u % 3]
                    for kc in range(8):
                        gather(W2[u % 2][kc][:, :], w2_rows, ix[:, kc:kc + 1], 32768, [ix], [W2[u % 2][kc]])

                def emit_T(u):
                    if u >= NU:
                        return
                    xs, xt_ = XS[u % 2], XTu[u % 2]
                    for a4 in range(4):
                        pt = pst()
                        for kc in range(8):
                            p.tr(pt[:, kc * 128:(kc + 1) * 128], xs[:, a4, kc * 128:(kc + 1) * 128], idb[:], R=[xs, idb], W=[pt])
                        p.cp("act", xt_[:, :, a4 * 128:(a4 + 1) * 128], pt[:].rearrange("p (k t) -> p k t", k=8), R=[pt], W=[xt_])

                def emit_mm1(u):
                    wg_, wl_ = W1G[u % 2], W1L[u % 2]
                    AT = ATs[u % 2]
                    xt_ = XTu[u % 2]
                    bg, bl = B1G[u % 3], B1L[u % 3]
                    for c in range(8):
                        j = cc[0] % 2
                        cc[0] += 1
                        GLt, SGt, HBt, L2t = GLs[j], SGs[j], HBs[j], L2s[j]
                        pg = psa()
                        for kc in range(8):
                            p.mm(pg[:], wg_[kc][:, c * 128:(c + 1) * 128], xt_[:, kc, :],
                                 start=(kc == 0), stop=(kc == 7), R=[wg_[kc], xt_], W=[pg])
                        pl = psa()
                        for kc in range(8):
                            p.mm(pl[:], wl_[kc][:, c * 128:(c + 1) * 128], xt_[:, kc, :],
                                 start=(kc == 0), stop=(kc == 7), R=[wl_[kc], xt_], W=[pl])
                        p.ts("dve", GLt[:], pg[:], bg[:, c:c + 1], 7.0, ALU.add, ALU.min, R=[pg, bg], W=[GLt])
                        p.act(SGt[:], GLt[:], AF.Sigmoid, R=[GLt], W=[SGt], scale=1.702)
                        p.act(HBt[:], pl[:], AF.Identity, R=[pl, bl], W=[HBt], bias=bl[:, c:c + 1])
                        p.ts("dve", L2t[:], HBt[:], 7.0, -7.0, ALU.min, ALU.max, R=[HBt], W=[L2t])
                        p.tt("dve", GLt[:], GLt[:], SGt[:], ALU.mult, R=[GLt, SGt], W=[GLt])
                        p.stt("dve", AT[:, c, :], L2t[:], 1.0, GLt[:], ALU.add, ALU.mult, R=[L2t, GLt], W=[AT])

                def emit_mm2(u):
                    AT = ATs[u % 2]
                    w2_ = W2[u % 2]
                    for t4 in range(4):
                        yo = YO[yc[0] % 2]
                        yc[0] += 1
                        for hf in range(2):
                            py = psa()
                            for c in range(8):
                                p.mm(py[:], AT[:, c, t4 * 128:(t4 + 1) * 128], w2_[c][:, hf * 512:(hf + 1) * 512],
                                     start=(c == 0), stop=(c == 7), R=[AT, w2_[c]], W=[py])
                            p.cp("act", yo[:, hf * 512:(hf + 1) * 512], py[:], R=[py], W=[yo])
                        r0 = ORD[u] * 512 + t4 * 128
                        p.dma("sp", Ys[r0:r0 + 128, :], yo[:], "st", R=[yo], W=[dYs])

                nun = NU if stage >= 5 else 0
                if nun:
                    prep_xs(0)
                    prep_w1(0)
                    prep_w2(0)
                    prep_xs(1)
                    prep_w1(1)
                    emit_T(0)
                    emit_mm1(0)
                    emit_T(1)
                for u in range(nun):
                    if u + 1 < nun:
                        emit_mm1(u + 1)
                    prep_xs(u + 2)
                    emit_T(u + 2)
                    emit_mm2(u)
                    prep_w1(u + 2)
                    prep_w2(u + 1)
                p.flush()

            with ExitStack() as ph:
                CWT = sb("CWTc", [32, 128], F32, ph)
                ACt = [sb("act%d" % i, [128, 1024], F32, ph) for i in range(2)]
                GB = [sb("gb%d" % i, [128, 1024], F32, ph) for i in range(8)]
                X2s = [sb("x2s%d" % i, [128, 1024], F32, ph) for i in range(2)]
                junkf = sb("junkfin", [128, 1024], BF16, ph)
                SSf = [sb("ssfin%d" % i, [128, 1], F32, ph) for i in range(2)]
                RSf = [sb("rsfin%d" % i, [128, 1], F32, ph) for i in range(2)]
                gc = [0]
                for t in range(16 if stage >= 5 else 0):
                    ac, x2, ssf, rsf = ACt[t % 2], X2s[t % 2], SSf[t % 2], RSf[t % 2]
                    p.dma("sp", x2[:], X1[t * 128:(t + 1) * 128, :], "ldx", R=[dX1], W=[x2])
                    gs = []
                    for k in range(4):
                        g_ = GB[gc[0] % 8]
                        gc[0] += 1
                        j = 4 * t + k
                        p.op("pool", lambda e, g_=g_, j=j: e.indirect_dma_start(
                            out=g_[:, :], out_offset=None, in_=Ys,
                            in_offset=bass.IndirectOffsetOnAxis(ap=SLI[:, j:j + 1], axis=0)), R=[dYs, SLI], W=[g_], dma="ind")
                        gs.append(g_)
                    pc = psa()
                    p.tr(pc[0:32, 0:128], CWF[:, t, :], idf[:], R=[CWFB[t], idf], W=[pc])
                    p.cp("act", CWT[:], pc[0:32, 0:128], R=[pc], W=[CWT])
                    for hf in range(2):
                        py = psa()
                        p.mm(py[:], CWT[:], b2s[:, hf * 512:(hf + 1) * 512], R=[CWT, b2s], W=[py])
                        p.cp("act", ac[:, hf * 512:(hf + 1) * 512], py[:], R=[py], W=[ac])
                    for k in range(4):
                        p.stt("dve", ac[:], gs[k][:], CW4[:, t, k:k + 1], ac[:], ALU.mult, ALU.add, R=[gs[k], CW4, ac], W=[ac])
                    p.tt("dve", ac[:], ac[:], GTF[:], ALU.mult, R=[ac, GTF], W=[ac])
                    p.tt("pool", x2[:], x2[:], ac[:], ALU.add, R=[x2, ac], W=[x2])
                    p.memset("pool", ssf[:], 0.0, W=[ssf])
                    p.act(junkf[:], x2[:], AF.Square, R=[x2], W=[junkf, ssf], accum_out=ssf[:])
                    p.ts("dve", rsf[:], ssf[:], 1.0 / D, EPS, ALU.mult, ALU.add, R=[ssf], W=[rsf])
                    p.act(rsf[:], rsf[:], AF.Sqrt, R=[rsf], W=[rsf])
                    p.op("dve", lambda e, rsf=rsf: e.reciprocal(rsf[:], rsf[:]), R=[rsf], W=[rsf])
                    p.stt("dve", ac[:], x2[:], rsf[:], GFIN[:], ALU.mult, ALU.mult, R=[x2, rsf, GFIN], W=[ac])
                    p.dma("sp", y[t * 128:(t + 1) * 128, :], ac[:], "st", R=[ac], W=[dY])
                p.flush()
    return nc


def _t5_buckets(dist):
    n = np.maximum(dist, 0)
    nf = np.maximum(n, 1).astype(np.float32)
    large = 16 + (np.log(nf / np.float32(16)) / np.float32(np.log(8.0)) * np.float32(16)).astype(np.int32)
    large = np.minimum(large, 31)
    return np.where(n < 16, n, large)


_NC_CACHE = {}


def kernel(x, c, w_ada, b_ada, g_mix, w_in, b_forget, sinks, rel_bias, w_proj_a, w_proj_b, w_out,
           g_ffn, w_router, b_router, w_e1, b_e1, w_e2, b_e2, g_final):
    f32 = np.float32
    A = lambda a: np.ascontiguousarray(np.asarray(a, dtype=f32))
    x2 = A(x)[0]
    xb = x2.reshape(128, 128, D)
    k = np.arange(128)[:, None]
    q = np.arange(128)[None, :]
    dist_halo = 128 + q - k
    dist_own = q - k
    rb = A(rel_bias)
    swa_bias = np.zeros((128, 2, 8, 128), f32)
    swa_bias[:, 0] = rb[_t5_buckets(dist_halo)].transpose(0, 2, 1)
    swa_bias[:, 1] = rb[_t5_buckets(dist_own)].transpose(0, 2, 1)
    swa_mask = np.zeros((128, 2, 128), f32)
    swa_mask[:, 0] = np.where((dist_halo >= 0) & (dist_halo < 128), 0.0, NEG)
    swa_mask[:, 1] = np.where((dist_own >= 0) & (dist_own < 128), 0.0, NEG)
    w1 = A(w_e1)[0]
    b1 = A(b_e1)[0]
    shared = {
        "x_all": x2,
        "c_t": A(np.asarray(c, f32).reshape(8, 128).T),
        "w_ada": A(w_ada)[0], "b_ada": A(b_ada).reshape(1, -1), "g_mix": A(g_mix).reshape(1, -1),
        "w_in": A(w_in)[0], "b_forget": A(b_forget).reshape(1, 8), "sinks": A(sinks).reshape(1, 8),
        "swa_bias": swa_bias.reshape(128, -1), "swa_mask": swa_mask.reshape(128, -1),
        "w_pa": A(w_proj_a)[0], "w_pb": A(w_proj_b)[0], "w_out": A(w_out)[0],
        "g_ffn": A(g_ffn).reshape(1, -1), "w_router": A(w_router)[0], "b_router": A(b_router).reshape(1, 32),
        "w1g": A(w1[:, :, 0::2]), "w1l": A(w1[:, :, 1::2]),
        "b1g_rows": A(b1[:, 0::2].reshape(32, 8, 128).transpose(0, 2, 1).reshape(4096, 8)),
        "b1l_rows": A(b1[:, 1::2].reshape(32, 8, 128).transpose(0, 2, 1).reshape(4096, 8)),
        "mconst": A(np.concatenate([np.tile((np.arange(48) * 512.0)[None, :], (128, 1)),
                                    (np.arange(8)[None, :] * 128.0 + np.arange(128)[:, None]),
                                    np.arange(128, dtype=f32)[:, None]], axis=1)),
        "tris": np.triu(np.ones((128, 128), f32), 1),
        "w2": A(w_e2)[0], "b2": A(b_e2)[0], "g_final": A(g_final).reshape(1, -1),
        "ident": np.eye(128, dtype=f32),
        "tri": np.triu(np.ones((128, 128), f32)),
    }
    in_maps = []
    for i in range(NCORES):
        own = xb[i::8]
        halo_ids = np.arange(16) * 8 + i - 1
        halo = np.zeros((16, 128, D), f32)
        for m_, j in enumerate(halo_ids):
            if j >= 0:
                halo[m_] = xb[j]
        kp = np.arange(128)[:, None, None]
        kb8 = np.arange(8)[None, :, None]
        qq = np.arange(128)[None, None, :]
        fm = np.where(kb8 * 128 + kp <= i * 128 + qq, BIG, NEG).astype(f32)
        meta = np.zeros((128, 24), f32)
        meta[:, i] = 1.0
        if i == 0:
            meta[:, 8] = NEG
        d = dict(shared)
        d["x_own"] = A(own.reshape(2048, D))
        d["x_halo"] = A(halo.reshape(2048, D))
        d["fmask"] = fm.reshape(128, -1)
        d["meta"] = meta
        in_maps.append(d)
    if "nc" not in _NC_CACHE:
        _NC_CACHE["nc"] = build(STAGE)
    nc = _NC_CACHE["nc"]
    res = run_bass_kernel_spmd(nc, in_maps, core_ids=list(range(NCORES)))
    if DEBUG:
        _NC_CACHE["res"] = res
    out = np.zeros((128, 128, D), f32)
    for i in range(NCORES):
        out[i::8] = res.results[i]["y"].reshape(16, 128, D)
    return out.reshape(1, S, D)
```
